# Optimizing a Trainium2 kernel written in Bass

```python
import jax
import jax.numpy as jnp
from jax import lax
import numpy as np

D_MODEL = 1024
BATCH = 16
SEQ = 2048
DEPTH = 4

GRID_W = 64
CTX_LEN = 256
HEAD_DIM = 64
ROT_FREQS = HEAD_DIM // 4
ROPE_THETA = 10000.0
NORM_EPS = 1e-5

POOL_WINDOWS = (2, 4, 8, 16)
POOL_GROUP = 64
POOL_WIDTH = POOL_GROUP * len(POOL_WINDOWS)
ATT_HEADS = 12
ATT_KV_HEADS = 3
ATT_GROUP = ATT_HEADS // ATT_KV_HEADS
ATT_WINDOW = 128
ATT_BLOCK = 128
ATT_Q = ATT_HEADS * HEAD_DIM
ATT_KV = ATT_KV_HEADS * HEAD_DIM
OFF_K = POOL_WIDTH + ATT_Q
OFF_V = OFF_K + ATT_KV
EVEN_IN = OFF_V + ATT_KV
EVEN_MIX = POOL_WIDTH + ATT_Q

RWKV_HEADS = 12
RWKV_WIDTH = RWKV_HEADS * HEAD_DIM
DECAY_LORA = 64
ICLR_LORA = 64
GATE_LORA = 160
GN_EPS = 64e-5
STATE_LO = RWKV_WIDTH
STATE_HI = 3 * RWKV_WIDTH + 2 * DECAY_LORA + 2 * ICLR_LORA
RWKV_IN = STATE_HI + GATE_LORA
FNET_GROUPS = 4
FNET_GROUP = 64
FNET_WIDTH = FNET_GROUPS * FNET_GROUP
ODD_IN = RWKV_IN + FNET_WIDTH
ODD_MIX = RWKV_WIDTH + FNET_WIDTH

N_EXPERTS = 32
TOP_K = 4
D_EXPERT = D_MODEL
SWIGLU_LIMIT = 7.0
SWIGLU_ALPHA = 1.702
MOE_BLOCK = 256

kernel_name = 'hybrid_pool_swa_rwkv7_fnet_moe_dit'


def rms_norm(x, g):
    xf = x.astype(jnp.float32)
    xf = xf * lax.rsqrt(jnp.mean(xf * xf, axis=-1, keepdims=True) + NORM_EPS)
    return (xf * g.astype(jnp.float32)).astype(x.dtype)


def modulate(h, shift, scale):
    return h * (1 + scale[:, None, :]) + shift[:, None, :]


def axial_rope_tables(n_tokens):
    rows = n_tokens // GRID_W
    row = jnp.repeat(jnp.arange(rows, dtype=jnp.float32), GRID_W)
    col = jnp.tile(jnp.arange(GRID_W, dtype=jnp.float32), rows)
    inv_freq = ROPE_THETA ** (-jnp.arange(ROT_FREQS, dtype=jnp.float32) / ROT_FREQS)
    ang = jnp.stack([row[:, None] * inv_freq, col[:, None] * inv_freq], axis=1)
    return jnp.cos(ang), jnp.sin(ang)


def apply_axial_rope(x, cos, sin):
    xs = x.astype(jnp.float32).reshape(x.shape[:-1] + (2, 2, ROT_FREQS))
    x1, x2 = xs[..., 0, :], xs[..., 1, :]
    c, s = cos[:, None], sin[:, None]
    out = jnp.stack([x1 * c - x2 * s, x2 * c + x1 * s], axis=-2)
    return out.reshape(x.shape).astype(x.dtype)


def multiscale_pool(u, pool_w, pool_scale):
    B, T, _ = u.shape
    uf = u.astype(jnp.float32).reshape(B, T, len(POOL_WINDOWS), POOL_GROUP)
    cs = jnp.pad(lax.cumsum(uf, axis=1), ((0, 0), (1, 0), (0, 0), (0, 0)))
    t = jnp.arange(T)
    means = []
    for gi, w in enumerate(POOL_WINDOWS):
        lo = jnp.clip(t - w // 2, 0, T)
        hi = jnp.clip(t - w // 2 + w, 0, T)
        win = cs[:, hi, gi] - cs[:, lo, gi]
        means.append(win / (hi - lo).astype(jnp.float32)[None, :, None])
    pooled = (jnp.stack(means, axis=2) - uf).astype(u.dtype)
    mixed = jnp.einsum('btgc,gcd->btgd', pooled, pool_w)
    return mixed.reshape(B, T, POOL_WIDTH) * pool_scale


def sink_softmax(scores, sink):
    full = jnp.concatenate([scores, jnp.broadcast_to(sink, scores.shape[:-1] + (1,))], axis=-1)
    return jax.nn.softmax(full, axis=-1)[..., :-1]


def latent_window_attention(q, k, v, kc, vc, sink):
    B, S = q.shape[:2]
    nb = S // ATT_BLOCK
    span = ATT_BLOCK + 2 * ATT_WINDOW
    scale = HEAD_DIM ** -0.5
    qb = q.reshape(B, nb, ATT_BLOCK, ATT_KV_HEADS, ATT_GROUP, HEAD_DIM)
    pad = ((0, 0), (ATT_WINDOW, ATT_WINDOW), (0, 0), (0, 0))
    kp, vp = jnp.pad(k, pad), jnp.pad(v, pad)
    sink_b = sink.astype(jnp.float32).reshape(1, ATT_KV_HEADS, ATT_GROUP, 1, 1)
    rel = jnp.arange(span)[None, :] - jnp.arange(ATT_BLOCK)[:, None]
    band = (rel >= 0) & (rel <= 2 * ATT_WINDOW)

    def block(j):
        qj = lax.dynamic_index_in_dim(qb, j, axis=1, keepdims=False)
        kj = lax.dynamic_slice_in_dim(kp, j * ATT_BLOCK, span, axis=1)
        vj = lax.dynamic_slice_in_dim(vp, j * ATT_BLOCK, span, axis=1)
        key_pos = j * ATT_BLOCK - ATT_WINDOW + jnp.arange(span)
        mask = band & ((key_pos >= 0) & (key_pos < S))[None, :]
        s_loc = jnp.einsum('bqkgd,bskd->bkgqs', qj, kj).astype(jnp.float32) * scale
        s_loc = jnp.where(mask, s_loc, -jnp.inf)
        s_ctx = jnp.einsum('bqkgd,bckd->bkgqc', qj, kc).astype(jnp.float32) * scale
        p = sink_softmax(jnp.concatenate([s_loc, s_ctx], axis=-1), sink_b).astype(v.dtype)
        o = (jnp.einsum('bkgqs,bskd->bqkgd', p[..., :span], vj)
             + jnp.einsum('bkgqc,bckd->bqkgd', p[..., span:], vc))
        return o.reshape(B, ATT_BLOCK, ATT_Q)

    out = lax.map(block, jnp.arange(nb))
    return jnp.moveaxis(out, 0, 1).reshape(B, S, ATT_Q)


def context_attention(qc, kc, vc, sink):
    B, C = qc.shape[:2]
    qg = qc.reshape(B, C, ATT_KV_HEADS, ATT_GROUP, HEAD_DIM)
    s = jnp.einsum('bqkgd,bckd->bkgqc', qg, kc).astype(jnp.float32) * HEAD_DIM ** -0.5
    p = sink_softmax(s, sink.astype(jnp.float32).reshape(1, ATT_KV_HEADS, ATT_GROUP, 1, 1)).astype(vc.dtype)
    return jnp.einsum('bkgqc,bckd->bqkgd', p, vc).reshape(B, C, ATT_Q)


def even_mixer(hx, hc, w_in, w_out, pool_w, pool_scale, sink, cos, sin, ctx_out):
    B, S, _ = hx.shape
    C = hc.shape[1]
    px = hx @ w_in
    qx = apply_axial_rope(px[..., POOL_WIDTH:OFF_K].reshape(B, S, ATT_HEADS, HEAD_DIM), cos, sin)
    kx = apply_axial_rope(px[..., OFF_K:OFF_V].reshape(B, S, ATT_KV_HEADS, HEAD_DIM), cos, sin)
    vx = px[..., OFF_V:].reshape(B, S, ATT_KV_HEADS, HEAD_DIM)
    pc = hc @ w_in if ctx_out else hc @ w_in[:, OFF_K:]
    kc = pc[..., -2 * ATT_KV:-ATT_KV].reshape(B, C, ATT_KV_HEADS, HEAD_DIM)
    vc = pc[..., -ATT_KV:].reshape(B, C, ATT_KV_HEADS, HEAD_DIM)
    att_x = latent_window_attention(qx, kx, vx, kc, vc, sink)
    pool_x = multiscale_pool(px[..., :POOL_WIDTH], pool_w, pool_scale)
    yx = jnp.concatenate([pool_x, att_x], axis=-1) @ w_out
    if not ctx_out:
        return yx, None
    qc = pc[..., POOL_WIDTH:OFF_K].reshape(B, C, ATT_HEADS, HEAD_DIM)
    att_c = context_attention(qc, kc, vc, sink)
    pool_c = multiscale_pool(pc[..., :POOL_WIDTH], pool_w, pool_scale)
    yc = jnp.concatenate([pool_c, att_c], axis=-1) @ w_out
    return yx, yc


def token_shift(u, mu):
    prev = jnp.pad(u[:, :-1], ((0, 0), (1, 0), (0, 0)))
    nxt = jnp.pad(u[:, 1:], ((0, 0), (0, 1), (0, 0)))
    return u + mu[0] * (prev - u) + mu[1] * (nxt - u)


def to_heads_tm(z):
    B, T, _ = z.shape
    return jnp.moveaxis(z.astype(jnp.float32).reshape(B, T, RWKV_HEADS, HEAD_DIM), 1, 0)


def rwkv_scan_inputs(fs, w0, w2, a0, a2, k_k, k_a):
    B, T, _ = fs.shape
    W = RWKV_WIDTH
    k, v = fs[..., :W], fs[..., W:2 * W]
    o_w = 2 * W
    o_a = o_w + 2 * DECAY_LORA
    kk = (k * k_k).astype(jnp.float32).reshape(B, T, RWKV_HEADS, HEAD_DIM)
    kk = kk / jnp.maximum(jnp.sqrt(jnp.sum(kk * kk, axis=-1, keepdims=True)), 1e-12)
    kk = kk.reshape(B, T, W)
    kf = k.astype(jnp.float32)
    dirs = []
    for d in range(2):
        wd = fs[..., o_w + d * DECAY_LORA:o_w + (d + 1) * DECAY_LORA]
        ad = fs[..., o_a + d * ICLR_LORA:o_a + (d + 1) * ICLR_LORA]
        w_log = -jax.nn.softplus(-(w0[d] + jnp.tanh(wd) @ w2[d]).astype(jnp.float32)) - 0.5
        decay = jnp.exp(-jnp.exp(w_log))
        a = jax.nn.sigmoid((a0[d] + ad @ a2[d]).astype(jnp.float32))
        k_d = kf * (1.0 + (a - 1.0) * k_a.astype(jnp.float32))
        dirs.append((decay, k_d, kk * a))
    return v, kk, dirs


def wkv_scan(state0, scan_in, d, r, reverse):
    v, kk, dirs = scan_in
    decay, k_d, b = dirs[d]
    seq = tuple(to_heads_tm(z) for z in (decay, k_d, v, kk, b))
    if r is not None:
        seq = seq + (to_heads_tm(r),)

    def step(S, inp):
        w_t, k_t, v_t, kk_t, b_t = inp[:5]
        sa = jnp.einsum('bhij,bhj->bhi', S, kk_t)
        S = S * w_t[:, :, None, :] - sa[..., None] * b_t[:, :, None, :] + v_t[..., None] * k_t[:, :, None, :]
        y = jnp.einsum('bhij,bhj->bhi', S, inp[5]) if len(inp) == 6 else None
        return S, y

    return lax.scan(step, state0, seq, reverse=reverse)


def rwkv_readout(y_tm, r, scan_in, g_cols, g2, r_k, gn_g, gn_b):
    v, _, dirs = scan_in
    y = jnp.moveaxis(y_tm, 0, 1)
    B, T = y.shape[:2]
    mean = jnp.mean(y, axis=-1, keepdims=True)
    var = jnp.mean(jnp.square(y - mean), axis=-1, keepdims=True)
    yn = ((y - mean) * lax.rsqrt(var + GN_EPS)).reshape(B, T, RWKV_WIDTH) * gn_g + gn_b
    rh = r.astype(jnp.float32).reshape(B, T, RWKV_HEADS, HEAD_DIM)
    vh = v.astype(jnp.float32).reshape(B, T, RWKV_HEADS, HEAD_DIM)
    coef = sum(jnp.sum(rh * dd[1].reshape(B, T, RWKV_HEADS, HEAD_DIM) * r_k, axis=-1, keepdims=True)
               for dd in dirs)
    out = yn + (coef * vh).reshape(B, T, RWKV_WIDTH)
    g = jax.nn.sigmoid(g_cols) @ g2
    return out.astype(r.dtype) * g


def fourier_mix(u):
    B, T, _ = u.shape
    uf = u.astype(jnp.float32).reshape(B, T, FNET_GROUPS, FNET_GROUP)
    return jnp.fft.fft2(uf, axes=(1, 3), norm='ortho').real.reshape(B, T, FNET_WIDTH).astype(u.dtype)


def odd_mixer(hx, hc, w_in, w_out, mu, w0, w2, a0, a2, g2, k_k, k_a, r_k, gn_g, gn_b, ctx_out):
    B = hx.shape[0]
    dir_params = (w0, w2, a0, a2, k_k, k_a)
    px = hx @ w_in
    fx = token_shift(px[..., :RWKV_IN], mu)
    x_in = rwkv_scan_inputs(fx[..., STATE_LO:STATE_HI], *dir_params)
    if ctx_out:
        pc = hc @ w_in
        fc = token_shift(pc[..., :RWKV_IN], mu)
        c_in = rwkv_scan_inputs(fc[..., STATE_LO:STATE_HI], *dir_params)
        rc = fc[..., :RWKV_WIDTH]
    else:
        fcs = token_shift(hc @ w_in[:, STATE_LO:STATE_HI], mu[:, STATE_LO:STATE_HI])
        c_in = rwkv_scan_inputs(fcs, *dir_params)
        rc = None
    rx = fx[..., :RWKV_WIDTH]
    state0 = jnp.zeros((B, RWKV_HEADS, HEAD_DIM, HEAD_DIM), jnp.float32)
    ys_x, ys_c = [], []
    for d, rev in enumerate((False, True)):
        s_c, y_c = wkv_scan(state0, c_in, d, rc, rev)
        _, y_x = wkv_scan(s_c, x_in, d, rx, rev)
        ys_x.append(y_x)
        ys_c.append(y_c)
    o_x = rwkv_readout(ys_x[0] + ys_x[1], rx, x_in, fx[..., STATE_HI:], g2, r_k, gn_g, gn_b)
    yx = jnp.concatenate([o_x, fourier_mix(px[..., RWKV_IN:])], axis=-1) @ w_out
    if not ctx_out:
        return yx, None
    o_c = rwkv_readout(ys_c[0] + ys_c[1], rc, c_in, fc[..., STATE_HI:], g2, r_k, gn_g, gn_b)
    yc = jnp.concatenate([o_c, fourier_mix(pc[..., RWKV_IN:])], axis=-1) @ w_out
    return yx, yc


def clamped_swiglu(gu):
    glu, lin = gu[..., ::2], gu[..., 1::2]
    glu = jnp.minimum(glu, SWIGLU_LIMIT)
    lin = jnp.clip(lin, -SWIGLU_LIMIT, SWIGLU_LIMIT)
    return glu * jax.nn.sigmoid(SWIGLU_ALPHA * glu) * (lin + 1)


def moe_ffn(h, router_w, router_b, w_gu, b_gu, w_dn, b_dn):
    n_tok, D = h.shape
    logits = (h @ router_w).astype(jnp.float32) + router_b.astype(jnp.float32)
    top_val, top_idx = lax.top_k(logits, TOP_K)
    gates = jax.nn.softmax(top_val, axis=-1)
    n_assign = n_tok * TOP_K
    flat_e = top_idx.reshape(-1).astype(jnp.int32)
    flat_tok = jnp.repeat(jnp.arange(n_tok, dtype=jnp.int32), TOP_K)
    flat_gate = gates.reshape(-1)
    order = jnp.argsort(flat_e)
    sorted_e, sorted_tok, sorted_gate = flat_e[order], flat_tok[order], flat_gate[order]
    counts = jax.ops.segment_sum(jnp.ones_like(flat_e), flat_e, num_segments=N_EXPERTS)
    starts = jnp.cumsum(counts) - counts
    padded = (counts + MOE_BLOCK - 1) // MOE_BLOCK * MOE_BLOCK
    pends = jnp.cumsum(padded)
    pstarts = pends - padded
    dest = pstarts[sorted_e] + (jnp.arange(n_assign, dtype=jnp.int32) - starts[sorted_e])
    n_rows = (-(-n_assign // MOE_BLOCK) + N_EXPERTS) * MOE_BLOCK
    n_blocks = n_rows // MOE_BLOCK
    row_tok = jnp.full((n_rows,), n_tok, jnp.int32).at[dest].set(sorted_tok)
    row_gate = jnp.zeros((n_rows,), jnp.float32).at[dest].set(sorted_gate)
    block_e = jnp.minimum(jnp.searchsorted(pends, jnp.arange(n_blocks) * MOE_BLOCK, side='right'),
                          N_EXPERTS - 1)
    h_pad = jnp.concatenate([h, jnp.zeros((1, D), h.dtype)], axis=0)

    def run(args):
        e, toks, gw = args
        xb = h_pad[toks]
        gu = xb @ w_gu[e] + b_gu[e]
        yb = clamped_swiglu(gu) @ w_dn[e] + b_dn[e]
        return yb * gw[:, None].astype(yb.dtype)

    y = lax.map(run, (block_e, row_tok.reshape(n_blocks, MOE_BLOCK), row_gate.reshape(n_blocks, MOE_BLOCK)))
    return jax.ops.segment_sum(y.reshape(n_rows, D), row_tok, num_segments=n_tok + 1)[:n_tok]


def setup_inputs(seed: int = 0) -> dict:
    key = jax.random.key(seed)
    keys = iter(jax.random.split(key, 40))
    D = D_MODEL
    n_even = (DEPTH + 1) // 2
    n_odd = DEPTH // 2

    def nrm(shape, std):
        return std * jax.random.normal(next(keys), shape, jnp.float32)

    def uni(shape, lo, hi):
        return jax.random.uniform(next(keys), shape, jnp.float32, lo, hi)

    return {
        'x': nrm((BATCH, SEQ, D), 1.0),
        'c': nrm((BATCH, D), 1.0),
        'ctx': nrm((BATCH, CTX_LEN, D), 1.0),
        'c_ctx': nrm((D,), 1.0),
        'ada_w': nrm((DEPTH, D, 6 * D), 0.5 * D ** -0.5),
        'ada_b': nrm((DEPTH, 6 * D), 0.02),
        'norm_mix_g': 1.0 + nrm((DEPTH, D), 0.02),
        'norm_ffn_g': 1.0 + nrm((DEPTH, D), 0.02),
        'ev_w_in': nrm((n_even, D, EVEN_IN), D ** -0.5),
        'ev_w_out': nrm((n_even, EVEN_MIX, D), EVEN_MIX ** -0.5),
        'pool_w': nrm((n_even, len(POOL_WINDOWS), POOL_GROUP, POOL_GROUP), POOL_GROUP ** -0.5),
        'pool_scale': 1.0 + nrm((n_even, POOL_WIDTH), 0.1),
        'att_sink': nrm((n_even, ATT_HEADS), 0.5),
        'od_w_in': nrm((n_odd, D, ODD_IN), D ** -0.5),
        'od_w_out': nrm((n_odd, ODD_MIX, D), ODD_MIX ** -0.5),
        'rw_mu': uni((n_odd, 2, RWKV_IN), 0.0, 0.5),
        'rw_w0': uni((n_odd, 2, RWKV_WIDTH), -6.0, -1.0),
        'rw_w2': nrm((n_odd, 2, DECAY_LORA, RWKV_WIDTH), 0.1),
        'rw_a0': nrm((n_odd, 2, RWKV_WIDTH), 0.5),
        'rw_a2': nrm((n_odd, 2, ICLR_LORA, RWKV_WIDTH), 0.1),
        'rw_g2': nrm((n_odd, GATE_LORA, RWKV_WIDTH), GATE_LORA ** -0.5),
        'rw_k_k': 0.85 + nrm((n_odd, RWKV_WIDTH), 0.05),
        'rw_k_a': 1.0 + nrm((n_odd, RWKV_WIDTH), 0.05),
        'rw_r_k': nrm((n_odd, RWKV_HEADS, HEAD_DIM), 0.1),
        'rw_gn_g': 1.0 + nrm((n_odd, RWKV_WIDTH), 0.02),
        'rw_gn_b': nrm((n_odd, RWKV_WIDTH), 0.01),
        'router_w': nrm((DEPTH, D, N_EXPERTS), D ** -0.5),
        'router_b': nrm((DEPTH, N_EXPERTS), 0.01),
        'exp_w_gu': nrm((DEPTH, N_EXPERTS, D, 2 * D_EXPERT), D ** -0.5),
        'exp_b_gu': nrm((DEPTH, N_EXPERTS, 2 * D_EXPERT), 0.01),
        'exp_w_dn': nrm((DEPTH, N_EXPERTS, D_EXPERT, D), D_EXPERT ** -0.5),
        'exp_b_dn': nrm((DEPTH, N_EXPERTS, D), 0.01),
        'final_g': 1.0 + nrm((D,), 0.02),
    }


def reference(x, c, ctx, c_ctx, ada_w, ada_b, norm_mix_g, norm_ffn_g, ev_w_in, ev_w_out, pool_w,
              pool_scale, att_sink, od_w_in, od_w_out, rw_mu, rw_w0, rw_w2, rw_a0, rw_a2, rw_g2,
              rw_k_k, rw_k_a, rw_r_k, rw_gn_g, rw_gn_b, router_w, router_b, exp_w_gu, exp_b_gu,
              exp_w_dn, exp_b_dn, final_g):
    B, S, D = x.shape
    C = ctx.shape[1]
    cos, sin = axial_rope_tables(S)
    h, hc = x, ctx
    for i in range(DEPTH):
        last = i == DEPTH - 1
        j = i // 2
        mod_x = jax.nn.silu(c) @ ada_w[i] + ada_b[i]
        mod_c = (jax.nn.silu(c_ctx) @ ada_w[i] + ada_b[i])[None]
        sh_m, sc_m, g_m, sh_f, sc_f, g_f = jnp.split(mod_x, 6, axis=-1)
        csh_m, csc_m, cg_m, csh_f, csc_f, cg_f = jnp.split(mod_c, 6, axis=-1)
        ax = modulate(rms_norm(h, norm_mix_g[i]), sh_m, sc_m)
        ac = modulate(rms_norm(hc, norm_mix_g[i]), csh_m, csc_m)
        if i % 2 == 0:
            yx, yc = even_mixer(ax, ac, ev_w_in[j], ev_w_out[j], pool_w[j], pool_scale[j], att_sink[j],
                                cos, sin, not last)
        else:
            yx, yc = odd_mixer(ax, ac, od_w_in[j], od_w_out[j], rw_mu[j], rw_w0[j], rw_w2[j], rw_a0[j],
                               rw_a2[j], rw_g2[j], rw_k_k[j], rw_k_a[j], rw_r_k[j], rw_gn_g[j],
                               rw_gn_b[j], not last)
        h = h + g_m[:, None, :] * yx
        fx = modulate(rms_norm(h, norm_ffn_g[i]), sh_f, sc_f).reshape(B * S, D)
        if last:
            out = moe_ffn(fx, router_w[i], router_b[i], exp_w_gu[i], exp_b_gu[i], exp_w_dn[i], exp_b_dn[i])
            h = h + g_f[:, None, :] * out.reshape(B, S, D)
        else:
            hc = hc + cg_m[:, None, :] * yc
            fc = modulate(rms_norm(hc, norm_ffn_g[i]), csh_f, csc_f).reshape(B * C, D)
            out = moe_ffn(jnp.concatenate([fx, fc], axis=0), router_w[i], router_b[i], exp_w_gu[i],
                          exp_b_gu[i], exp_w_dn[i], exp_b_dn[i])
            h = h + g_f[:, None, :] * out[:B * S].reshape(B, S, D)
            hc = hc + cg_f[:, None, :] * out[B * S:].reshape(B, C, D)
    return rms_norm(h, final_g)
```

```python
import numpy as np
from contextlib import ExitStack
import concourse.bass as bass
import concourse.mybir as mybir
from concourse.bass_utils import run_bass_kernel_spmd

F32 = mybir.dt.float32
BF16 = mybir.dt.bfloat16
I32 = mybir.dt.int32
ALU = mybir.AluOpType
ACT = mybir.ActivationFunctionType
AX = mybir.AxisListType

SAME_ENGINE_SYNC = True


class Res:
    __slots__ = ("name", "t", "lw", "rd")

    def __init__(self, name, t=None):
        self.name = name
        self.t = t
        self.lw = None
        self.rd = {}

    def __getitem__(self, idx):
        return self.t[idx]


class Sched:
    NDMA = {"sp": 40, "pool": 16, "act": 8}

    def __init__(self, nc):
        self.nc = nc
        self.stack = ExitStack()
        self.E = {"pe": nc.tensor, "dve": nc.vector, "act": nc.scalar, "pool": nc.gpsimd, "sp": nc.sync}
        self.sem = {}
        self.val = {}
        for k in ("pe", "dve", "act", "pool"):
            self.sem[k] = self.stack.enter_context(nc.semaphore("s_" + k))
            self.val[k] = 0
        self.rr = {}
        for q, n in self.NDMA.items():
            self.rr[q] = 0
            for i in range(n):
                self.sem[(q, i)] = self.stack.enter_context(nc.semaphore(f"d_{q}{i}"))
                self.val[(q, i)] = 0
        self.known = {e: {} for e in self.E}
        self.n_inst = 0
        self.n_wait = 0

    def sbuf(self, name, shape, dtype, st=None):
        self.uid = getattr(self, "uid", 0) + 1
        t = (st or self.stack).enter_context(self.nc.sbuf_tensor(f"{name}_{self.uid}", list(shape), dtype))
        return Res(name, t)

    def barrier(self):
        for eng, e in self.E.items():
            kn = self.known[eng]
            for k, v in self.val.items():
                if v > 0 and kn.get(k, 0) < v and k != eng:
                    e.wait_ge(self.sem[k], v)
                    kn[k] = v
                    self.n_wait += 1

    def psum(self, name, shape, dtype=F32):
        t = self.stack.enter_context(self.nc.psum_tensor(name, list(shape), dtype))
        return Res(name, t)

    def dram(self, name, shape, dtype, kind="Internal"):
        t = self.nc.dram_tensor(name, list(shape), dtype, kind=kind)
        return Res(name, t.ap())

    def view(self, name):
        return Res(name, None)

    def _need(self, eng, reads, writes):
        need = {}

        def add(ev):
            if ev is None:
                return
            k, v = ev
            if need.get(k, 0) < v:
                need[k] = v
        for r in reads:
            add(r.lw)
        for w in writes:
            add(w.lw)
            for k, v in w.rd.items():
                add((k, v))
        e = self.E[eng]
        kn = self.known[eng]
        for k, v in need.items():
            if k == eng:
                if not SAME_ENGINE_SYNC or eng == "pe":
                    continue
            if kn.get(k, 0) < v:
                e.wait_ge(self.sem[k], v)
                kn[k] = v
                self.n_wait += 1

    def _mark(self, ev, reads, writes):
        k, v = ev
        for r in reads:
            if r.rd.get(k, 0) < v:
                r.rd[k] = v
        for w in writes:
            w.lw = ev
            w.rd = {}

    def op(self, eng, fn, reads=(), writes=()):
        self._need(eng, reads, writes)
        inst = fn(self.E[eng])
        self.val[eng] += 1
        inst.then_inc(self.sem[eng], 1)
        self._mark((eng, self.val[eng]), reads, writes)
        self.n_inst += 1
        return inst

    def dma(self, q, out, in_, reads=(), writes=(), **kw):
        e = self.E[q]
        i = self.rr[q]
        self.rr[q] = (i + 1) % self.NDMA[q]
        k = (q, i)
        kn = self.known[q]
        if kn.get(k, 0) < self.val[k]:
            e.wait_ge(self.sem[k], self.val[k])
            kn[k] = self.val[k]
            self.n_wait += 1
        self._need(q, reads, writes)
        inst = e.dma_start(out=out, in_=in_, **kw)
        self.val[k] += 16
        inst.then_inc(self.sem[k], 16)
        self._mark((k, self.val[k]), reads, writes)
        self.n_inst += 1
        return inst

    def finish(self):
        e = self.E["sp"]
        kn = self.known["sp"]
        for k, v in self.val.items():
            if v > 0 and kn.get(k, 0) < v:
                e.wait_ge(self.sem[k], v)
                kn[k] = v
        self.stack.close()


D = 1024
KC = 8
NB = 2
CTX = 256
SEQ = 2048
NTB = CTX + SEQ
NT = NB * NTB
DEPTH = 4
NE = 32
EPS = 1e-5
NCORES = 8


def token_tiles():
    tl = []
    for b in range(NB):
        tl.append((b * NTB, CTX, 2, b, True))
        for i in range(SEQ // 512):
            tl.append((b * NTB + CTX + 512 * i, 512, b, b, False))
    return tl


class Prog:
    def __init__(self, layers=range(DEPTH), do_mixer=True, do_ffn=True, do_final=True, dbg=None):
        self.dbg = dbg or {}
        self.layers = list(layers)
        self.do_mixer = do_mixer
        self.do_ffn = do_ffn
        self.do_final = do_final
        self.nc = bass.Bass("TRN2", target_bir_lowering=False)
        self.S = Sched(self.nc)
        self.inputs = {}
        self._build()

    def inp(self, name, shape, dtype=F32):
        r = self.S.dram(name, shape, dtype, kind="ExternalInput")
        self.inputs[name] = r
        return r

    def _build(self):
        S = self.S
        self.H = self.inp("h0", [128, KC, NT])
        self.Hs = S.dram("Hs", [128, KC, NT], F32)
        self.OUT = S.dram("out", [128, KC, NT], F32, kind="ExternalOutput")
        self.FX = S.dram("FX", [128, KC, NT], BF16)
        self.cT = self.inp("cT", [128, KC, 4])
        NL = len(self.layers)
        self.lidx = {li: i for i, li in enumerate(self.layers)}
        self.ada_w = self.inp("ada_w", [NL, D, 6 * D])
        self.ada_b4 = self.inp("ada_b4", [NL, 128, 48, 4])
        self.ng = self.inp("ng", [NL, 2, 128, KC])
        self.router_w = self.inp("router_w", [NL, 128, KC, NE])
        self.router_b = self.inp("router_b", [NL, 128, NE])
        NEX = 1 if self.dbg.get("nomoe_w") else NE
        self.w_gu = self.inp("w_gu", [NL, NEX, D, 2 * D])
        self.b_gu = self.inp("b_gu", [NL, 128, NEX, 16])
        self.w_dn = self.inp("w_dn", [NL, NEX, D, D])
        self.b_dn = self.inp("b_dn", [NL, NEX, D])
        self.final_g = self.inp("final_g", [128, KC])
        NEV = max(1, len([l for l in self.layers if l % 2 == 0]))
        self.eidx = {l: i for i, l in enumerate([l for l in self.layers if l % 2 == 0])}
        self.ev_w = self.inp("ev_w", [NEV, D, 2752])
        self.ev_wout = self.inp("ev_wout", [NEV, D, D])
        self.pw_blk = self.inp("pw_blk", [NEV, 2, 128, 128])
        self.pscale = self.inp("pscale", [NEV, 128, 2])
        self.sinkcol = self.inp("sinkcol", [NEV, 128, 12])
        self.ropeC = self.inp("ropeC", [128, SEQ])
        self.ropeS = self.inp("ropeS", [128, SEQ])
        self.amask = self.inp("amask", [3, 128, 640])
        self.inv_x = self.inp("inv_x", [128, 2, SEQ])
        self.inv_c = self.inp("inv_c", [128, 2, CTX])
        NOD = max(1, len([l for l in self.layers if l % 2 == 1]))
        self.oidx = {l: i for i, l in enumerate([l for l in self.layers if l % 2 == 1])}
        self.od_w = self.inp("od_w", [NOD, D, 2976])
        self.od_wout = self.inp("od_wout", [NOD, D, D])
        self.mu_l = self.inp("mu_l", [NOD, 128, 22, 2])
        self.rw_w2 = self.inp("rw_w2", [NOD, 128, 768])
        self.rw_a2 = self.inp("rw_a2", [NOD, 128, 768])
        self.rw_g2 = self.inp("rw_g2", [NOD, 160, 768])
        self.rw_pv = self.inp("rw_pv", [NOD, 128, 6, 9])
        self.blk1 = self.inp("blk1", [128, 128])
        self.rmask = self.inp("rmask", [4, 128, 128])
        self.dft64 = self.inp("dft64", [2, 128, 128], BF16)
        self.dftx = self.inp("dftx", [2, 128, SEQ // 128, SEQ], BF16)
        self.dftc = self.inp("dftc", [2, 128, CTX // 128, CTX], BF16)
        self.F = S.dram("Fs", [128, 22, NT], BF16)
        self.FN = S.dram("FNs", [128, 2, NT], BF16)
        self.Q = S.dram("Qs", [128, 6, NT], BF16)
        self.K2 = S.dram("K2s", [128, 3, NT], BF16)
        self.V = S.dram("Vs", [128, NT // 128, 192], BF16)
        self.U = S.dram("Us", [128, 2, NT], F32)
        self.MIX = S.dram("MIXs", [128, KC, NT], BF16)
        self.sel = self.inp("sel", [NE, NE * 128])
        self.ident_in = self.inp("ident", [128, 128])

        self.ps2 = [S.psum(f"psd{i}", [128, 1024], F32) for i in range(4)]
        self.ps = []
        for i in range(4):
            for hh in range(2):
                self.ps.append(Res(f"ps{2 * i + hh}", self.ps2[i].t[:, hh * 512:(hh + 1) * 512]))
        self.ps_set = list(range(8))
        self.ps_rr = 0
        self.ones_bf = S.sbuf("ones_bf", [128, 128], BF16)
        S.op("pool", lambda e: e.memset(self.ones_bf[:], 1.0), writes=[self.ones_bf])
        self.ident = S.sbuf("ident", [128, 128], F32)
        S.dma("sp", self.ident[:], self.ident_in.t[:, :], reads=[self.ident_in], writes=[self.ident])
        self.silu_c = S.sbuf("silu_c", [128, KC, 4], F32)
        S.dma("sp", self.silu_c[:], self.cT.t[:, :, :], reads=[self.cT], writes=[self.silu_c])
        S.op("act", lambda e: e.activation(self.silu_c[:], self.silu_c[:], ACT.Silu),
             reads=[self.silu_c], writes=[self.silu_c])
        self.mod = S.sbuf("mod", [128, 48, 4], F32)
        self.gs = S.sbuf("gs", [128, 2, KC, 4], F32)
        self.ngs = S.sbuf("ngs", [128, 2, KC], F32)
        self.gneps = S.sbuf("gneps", [128, 1], F32)
        S.op("pool", lambda e: e.memset(self.gneps[:], 64e-5), writes=[self.gneps])

        first = True
        for li_ in self.layers:
            li = self.lidx[li_]
            self.adaln(li)
            src = self.H if first else self.Hs
            if self.do_mixer:
                if li_ % 2 == 0:
                    self.even_mixer(self.eidx[li_], src)
                else:
                    self.odd_mixer(self.oidx[li_], src)
                first = False
                src = self.Hs
            if self.do_ffn:
                st_ffn = ExitStack()
                self.gatesT = S.sbuf("gatesT", [NE, NT], F32, st_ffn)
                last = (li_ == DEPTH - 1)
                grp = None
                if self.dbg.get("norm", True):
                    self.norm_mod(li, 1, src, router=True, skip_ctx=False)
                if self.dbg.get("moe", True):
                    self.moe(li, src, grp)
                    first = False
                S.barrier()
                st_ffn.close()
        if self.do_final:
            self.final_norm(self.H if first else self.Hs)
        else:
            src = self.H if first else self.Hs
            for c in range(KC):
                S.dma("sp", self.OUT.t[:, c, :], src.t[:, c, :], reads=[src], writes=[self.OUT])
        S.finish()

    def next_ps(self):
        self.ps_rr = (self.ps_rr + 1) % len(self.ps_set)
        return self.ps[self.ps_set[self.ps_rr]]

    def adaln(self, li):
        S = self.S
        with ExitStack() as st:
            wbuf = [S.sbuf(f"adaw{i}", [128, KC, 512], F32, st) for i in range(2)]
            adab = S.sbuf("adab", [128, 48, 4], F32, st)
            S.dma("sp", adab[:], self.ada_b4.t[li], reads=[self.ada_b4], writes=[adab])
            S.dma("sp", self.ngs[:], self.ng.t[li].rearrange("a p c -> p a c"), reads=[self.ng], writes=[self.ngs])
            p = self.next_ps()
            for blk in range(12):
                w = wbuf[blk % 2]
                S.dma("sp", w[:], self.ada_w.t[li, :, blk * 512:(blk + 1) * 512].rearrange("(k p) n -> p k n", p=128),
                      reads=[self.ada_w], writes=[w])
                for mm in range(4):
                    m = blk * 4 + mm
                    for k in range(KC):
                        S.op("pe", lambda e: e.matmul(p[:, m * 4:(m + 1) * 4], w[:, k, mm * 128:(mm + 1) * 128],
                                                      self.silu_c[:, k, :], start=(k == 0), stop=(k == KC - 1)),
                             reads=[w, self.silu_c], writes=[p])
            S.op("dve", lambda e: e.tensor_tensor(self.mod[:].rearrange("p a b -> p (a b)"), p[:, 0:192],
                                                  adab[:].rearrange("p a b -> p (a b)"), ALU.add),
                 reads=[p, adab], writes=[self.mod])
            for which, base in ((0, 8), (1, 32)):
                for c in range(KC):
                    S.op("dve", lambda e: e.tensor_scalar(self.gs[:, which, c, :], self.mod[:, base + c, :], 1.0,
                                                          self.ngs[:, which, c:c + 1], ALU.add, ALU.mult),
                         reads=[self.mod, self.ngs], writes=[self.gs])
            S.barrier()

    def norm_mod_tile(self, which, h, t0, n, col, out_bf, out_f32=None, ooff=0):
        S = self.S
        sq = self.nm_sq
        S.op("act", lambda e: e.activation(sq[:, :, 0:n], h[:, :, 0:n], ACT.Square), reads=[h], writes=[sq])
        p = self.next_ps()
        for k in range(KC):
            S.op("pe", lambda e: e.matmul(p[:, 0:n], self.ones_bf[:], sq[:, k, 0:n], start=(k == 0), stop=(k == KC - 1)),
                 reads=[self.ones_bf, sq], writes=[p])
        rs = self.nm_rs
        S.op("act", lambda e: e.activation(rs[:, 0:n], p[:, 0:n], ACT.Sqrt, bias=self.eps_t[:, 0:1], scale=1.0 / D),
             reads=[p, self.eps_t], writes=[rs])
        S.op("dve", lambda e: e.reciprocal(rs[:, 0:n], rs[:, 0:n]), reads=[rs], writes=[rs])
        shb = 0 if which == 0 else 24
        for c in range(KC):
            tmp = self.nm_tmp[c % 2]
            S.op("dve", lambda e: e.scalar_tensor_tensor(tmp[:, 0:n], h[:, c, 0:n], self.gs[:, which, c, col:col + 1],
                                                         rs[:, 0:n], ALU.mult, ALU.mult),
                 reads=[h, self.gs, rs], writes=[tmp])
            S.op("act", lambda e: e.activation(out_bf[:, c, ooff:ooff + n], tmp[:, 0:n], ACT.Identity,
                                               bias=self.mod[:, shb + c, col:col + 1]),
                 reads=[tmp, self.mod], writes=[out_bf])
            if out_f32 is not None:
                S.op("pool", lambda e: e.tensor_scalar(out_f32[:, c, 0:n], tmp[:, 0:n], self.mod[:, shb + c, col:col + 1],
                                                       None, ALU.add),
                     reads=[tmp, self.mod], writes=[out_f32])

    def nm_alloc(self, st):
        S = self.S
        self.nm_sq = S.sbuf("nm_sq", [128, KC, 512], BF16, st)
        self.nm_rs = S.sbuf("nm_rs", [128, 512], F32, st)
        self.nm_tmp = [S.sbuf(f"nm_tmp{i}", [128, 512], F32, st) for i in range(2)]
        self.eps_t = S.sbuf("eps_t", [128, 1], F32, st)
        S.op("pool", lambda e: e.memset(self.eps_t[:], EPS), writes=[self.eps_t])

    def norm_mod(self, li, which, src, router, skip_ctx=False):
        S = self.S
        st = ExitStack()
        self.nm_alloc(st)
        hb = [S.sbuf(f"nmh{i}", [128, KC, 512], F32, st) for i in range(2)]
        ob = [S.sbuf(f"nmo{i}", [128, KC, 512], BF16, st) for i in range(2)]
        of = [S.sbuf(f"nmf{i}", [128, KC, 512], F32, st) for i in range(2)]
        rw = S.sbuf("rw", [128, KC, NE], F32, st)
        rb = S.sbuf("rb", [128, NE], F32, st)
        S.dma("sp", rw[:], self.router_w.t[li], reads=[self.router_w], writes=[rw])
        S.dma("sp", rb[:], self.router_b.t[li], reads=[self.router_b], writes=[rb])
        lg = S.sbuf("lg", [128, NE], F32, st)
        m8 = S.sbuf("m8", [128, 8], F32, st)
        nmx = S.sbuf("nmx", [128, 1], F32, st)
        msk = S.sbuf("msk", [128, NE], F32, st)
        ex = S.sbuf("ex", [128, NE], F32, st)
        ssum = S.sbuf("ssum", [128, 1], F32, st)
        for ti, (t0, n, col, b, isc) in enumerate(token_tiles()):
            if isc and skip_ctx:
                continue
            h = hb[ti % 2]
            o = ob[ti % 2]
            f = of[ti % 2]
            S.dma("sp", h[:, :, 0:n], src.t[:, :, t0:t0 + n], reads=[src], writes=[h])
            self.norm_mod_tile(which, h, t0, n, col, o, f)
            S.dma("sp", self.FX.t[:, :, t0:t0 + n], o[:, :, 0:n], reads=[o], writes=[self.FX])
            for s in range(n // 128):
                p = self.next_ps()
                for k in range(KC):
                    S.op("pe", lambda e: e.matmul(p[:, 0:NE], f[:, k, s * 128:(s + 1) * 128], rw[:, k, :],
                                                  start=(k == 0), stop=(k == KC - 1)),
                         reads=[f, rw], writes=[p])
                S.op("dve", lambda e: e.tensor_tensor(lg[:], p[:, 0:NE], rb[:], ALU.add), reads=[p, rb], writes=[lg])
                S.op("dve", lambda e: e.max(m8[:], lg[:]), reads=[lg], writes=[m8])
                S.op("dve", lambda e: e.tensor_scalar(msk[:], lg[:], m8[:, 3:4], None, ALU.is_ge), reads=[lg, m8], writes=[msk])
                S.op("dve", lambda e: e.tensor_scalar(nmx[:], m8[:, 0:1], -1.0, None, ALU.mult), reads=[m8], writes=[nmx])
                S.op("act", lambda e: e.activation(ex[:], lg[:], ACT.Exp, bias=nmx[:, 0:1]), reads=[lg, nmx], writes=[ex])
                S.op("dve", lambda e: e.tensor_tensor(ex[:], ex[:], msk[:], ALU.mult), reads=[ex, msk], writes=[ex])
                S.op("dve", lambda e: e.reduce_sum(ssum[:], ex[:], AX.X), reads=[ex], writes=[ssum])
                S.op("dve", lambda e: e.reciprocal(ssum[:], ssum[:]), reads=[ssum], writes=[ssum])
                S.op("dve", lambda e: e.tensor_scalar(ex[:], ex[:], ssum[:, 0:1], None, ALU.mult), reads=[ex, ssum], writes=[ex])
                p2 = self.next_ps()
                S.op("pe", lambda e: e.transpose(p2[0:NE, 0:128], ex[:], self.ident[:]), reads=[ex, self.ident], writes=[p2])
                tt = t0 + s * 128
                S.op("act", lambda e: e.activation(self.gatesT[:, tt:tt + 128], p2[0:NE, 0:128], ACT.Identity),
                     reads=[p2], writes=[self.gatesT])
        S.barrier()
        st.close()

    def even_proj(self, je, src):
        S = self.S
        st = ExitStack()
        self.nm_alloc(st)
        W = S.sbuf("evW", [128, KC, 2752], BF16, st)
        for k in range(KC):
            S.dma("pool", W[:, k, :], self.ev_w.t[je, k * 128:(k + 1) * 128, :], reads=[self.ev_w], writes=[W])
        rC = S.sbuf("ropeC", [128, SEQ], F32, st)
        rS = S.sbuf("ropeS", [128, SEQ], F32, st)
        S.dma("sp", rC[:], self.ropeC.t[:, :], reads=[self.ropeC], writes=[rC])
        S.dma("sp", rS[:], self.ropeS.t[:, :], reads=[self.ropeS], writes=[rS])
        hb = [S.sbuf(f"eph{i}", [128, KC, 512], F32, st) for i in range(2)]
        ob = [S.sbuf(f"epo{i}", [128, KC, 512], BF16, st) for i in range(2)]
        r1 = [S.sbuf(f"epr1{i}", [128, 512], F32, st) for i in range(2)]
        r2 = [S.sbuf(f"epr2{i}", [128, 512], F32, st) for i in range(2)]
        qo = [S.sbuf(f"epq{i}", [128, 512], BF16, st) for i in range(3)]
        uo = [S.sbuf(f"epu{i}", [128, 512], F32, st) for i in range(2)]
        vo = [S.sbuf(f"epv{i}", [128, 192], BF16, st) for i in range(2)]
        qi = 0
        for ti, (t0, n, col, b, isc) in enumerate(token_tiles()):
            h = hb[ti % 2]
            o = ob[ti % 2]
            S.dma("sp", h[:, :, 0:n], src.t[:, :, t0:t0 + n], reads=[src], writes=[h])
            self.norm_mod_tile(0, h, t0, n, col, o)

            def proj(p, off):
                for k in range(KC):
                    S.op("pe", lambda e: e.matmul(p[:, 0:n], W[:, k, off:off + 128], o[:, k, 0:n],
                                                  start=(k == 0), stop=(k == KC - 1)), reads=[W, o], writes=[p])
            for c in range(2):
                p = self.next_ps()
                proj(p, c * 128)
                u = uo[c]
                S.op("act", lambda e: e.activation(u[:, 0:n], p[:, 0:n], ACT.Identity), reads=[p], writes=[u])
                S.dma("sp", self.U.t[:, c, t0:t0 + n], u[:, 0:n], reads=[u], writes=[self.U])
            items = [(self.Q, m, 256 + m * 128, 1024 + m * 128) for m in range(6)] + \
                    [(self.K2, g, 1792 + g * 128, 2176 + g * 128) for g in range(3)]
            for (dst, dch, off, offsw) in items:
                q = qo[qi % 3]
                qi += 1
                p = self.next_ps()
                proj(p, off)
                if isc:
                    S.op("act", lambda e: e.activation(q[:, 0:n], p[:, 0:n], ACT.Identity), reads=[p], writes=[q])
                else:
                    p2 = self.next_ps()
                    proj(p2, offsw)
                    pos0 = t0 - b * NTB - CTX
                    a1 = r1[qi % 2]
                    a2 = r2[qi % 2]
                    S.op("dve", lambda e: e.tensor_tensor(a1[:, 0:n], p[:, 0:n], rC[:, pos0:pos0 + n], ALU.mult),
                         reads=[p, rC], writes=[a1])
                    S.op("dve", lambda e: e.tensor_tensor(a2[:, 0:n], p2[:, 0:n], rS[:, pos0:pos0 + n], ALU.mult),
                         reads=[p2, rS], writes=[a2])
                    S.op("pool", lambda e: e.tensor_tensor(q[:, 0:n], a1[:, 0:n], a2[:, 0:n], ALU.add),
                         reads=[a1, a2], writes=[q])
                S.dma("sp", dst.t[:, dch, t0:t0 + n], q[:, 0:n], reads=[q], writes=[dst])
            for s_ in range(n // 128):
                p = self.next_ps()
                for k in range(KC):
                    S.op("pe", lambda e: e.matmul(p[:, 0:192], o[:, k, s_ * 128:(s_ + 1) * 128], W[:, k, 2560:2752],
                                                  start=(k == 0), stop=(k == KC - 1)), reads=[W, o], writes=[p])
                v = vo[s_ % 2]
                S.op("act", lambda e: e.activation(v[:], p[:, 0:192], ACT.Identity), reads=[p], writes=[v])
                S.dma("sp", self.V.t[:, (t0 + s_ * 128) // 128, :], v[:], reads=[v], writes=[self.V])
        S.barrier()
        st.close()

    def even_attn(self, je):
        S = self.S
        st = ExitStack()
        self.ps_set = [6, 7]
        scp = [Res("scA", self.ps2[0].t), Res("scB", self.ps2[1].t)]
        masks = S.sbuf("amask", [128, 3, 640], F32, st)
        S.dma("sp", masks[:], self.amask.t.rearrange("v p n -> p v n"), reads=[self.amask], writes=[masks])
        sink = S.sbuf("sink", [128, 12], F32, st)
        S.dma("sp", sink[:], self.sinkcol.t[je], reads=[self.sinkcol], writes=[sink])
        Qb = S.sbuf("Qb", [128, 6, NTB], BF16, st)
        Kb = S.sbuf("Kb", [128, 3, NTB], BF16, st)
        Vb = S.sbuf("Vb", [128, NTB // 128, 192], BF16, st)
        att = S.sbuf("attb", [128, 6, NTB], BF16, st)
        ssb = [S.sbuf(f"ssb{i}", [128, 640], F32, st) for i in range(2)]
        eeb = [S.sbuf(f"eeb{i}", [128, 640], F32, st) for i in range(2)]
        pnb = [S.sbuf(f"pnb{i}", [128, 640], F32, st) for i in range(2)]
        ptb = [S.sbuf(f"ptb{i}", [128, 640], BF16, st) for i in range(2)]
        sm = [[S.sbuf(f"sm{i}_{j}", [128, 1], F32, st) for j in range(5)] for i in range(2)]
        tpp = [Res("tppA", self.ps2[2].t), Res("tppB", self.ps2[3].t)]

        def item_gen(h, blk, slot):
            g = h // 4
            hp = (h % 2) * 64
            qc = h // 2
            if blk < 0:
                q0 = (blk + 2) * 128
                segs = [(0, 256, 0)]
                mv = None
            else:
                q0 = CTX + blk * 128
                kb = [x for x in (blk - 1, blk, blk + 1) if 0 <= x < SEQ // 128]
                segs = [(0, 256, 0)] + [(CTX + x * 128, 128, 256 + i * 128) for i, x in enumerate(kb)]
                mv = 0 if blk == 0 else (2 if blk == SEQ // 128 - 1 else 1)
            ncol = sum(x[1] for x in segs)
            sc = scp[slot]
            tp = tpp[slot]
            ss = ssb[slot]
            ee = eeb[slot]
            pn = pnb[slot]
            pt = ptb[slot]
            mx, nmx, rsum, es, den = sm[slot]
            for (k0, kn, c0) in segs:
                S.op("pe", lambda e: e.matmul(sc[:, c0:c0 + kn], Qb[hp:hp + 64, qc, q0:q0 + 128],
                                              Kb[hp:hp + 64, g, k0:k0 + kn], start=True, stop=True),
                     reads=[Qb, Kb], writes=[sc])
            yield
            if mv is None:
                S.op("act", lambda e: e.activation(ss[:, 0:ncol], sc[:, 0:ncol], ACT.Identity), reads=[sc], writes=[ss])
            else:
                S.op("dve", lambda e: e.tensor_tensor(ss[:, 0:ncol], sc[:, 0:ncol], masks[:, mv, 0:ncol], ALU.add),
                     reads=[sc, masks], writes=[ss])
            S.op("dve", lambda e: e.reduce_max(mx[:], ss[:, 0:ncol], AX.X), reads=[ss], writes=[mx])
            S.op("dve", lambda e: e.tensor_scalar(nmx[:], mx[:], -0.125, None, ALU.mult), reads=[mx], writes=[nmx])
            yield
            S.op("act", lambda e: e.activation(ee[:, 0:ncol], ss[:, 0:ncol], ACT.Exp, bias=nmx[:, 0:1], scale=0.125,
                                               accum_out=rsum[:, 0:1]), reads=[ss, nmx], writes=[ee, rsum])
            S.op("act", lambda e: e.activation(es[:], nmx[:], ACT.Exp, bias=sink[:, h:h + 1]),
                 reads=[nmx, sink], writes=[es])
            yield
            S.op("dve", lambda e: e.tensor_tensor(den[:], rsum[:], es[:], ALU.add), reads=[rsum, es], writes=[den])
            S.op("dve", lambda e: e.reciprocal(den[:], den[:]), reads=[den], writes=[den])
            yield
            S.op("act", lambda e: e.activation(pn[:, 0:ncol], ee[:, 0:ncol], ACT.Copy, scale=den[:, 0:1]),
                 reads=[ee, den], writes=[pn])
            yield
            nbk = ncol // 128
            for i in range(nbk):
                S.op("pe", lambda e: e.transpose(tp[:, i * 128:(i + 1) * 128], pn[:, i * 128:(i + 1) * 128], self.ident[:]),
                     reads=[pn, self.ident], writes=[tp])
            yield
            S.op("dve", lambda e: e.tensor_copy(pt[:, 0:ncol], tp[:, 0:ncol]), reads=[tp], writes=[pt])
            yield
            kts = []
            for (k0, kn, c0) in segs:
                for j in range(kn // 128):
                    kts.append(k0 // 128 + j)
            for i, kt in enumerate(kts):
                S.op("pe", lambda e: e.matmul(sc[hp:hp + 64, 0:128], Vb[:, kt, g * 64:(g + 1) * 64],
                                              pt[:, i * 128:(i + 1) * 128], start=(i == 0), stop=(i == nbk - 1)),
                     reads=[Vb, pt], writes=[sc])
            yield
            S.op("act", lambda e: e.activation(att[hp:hp + 64, qc, q0:q0 + 128], sc[hp:hp + 64, 0:128], ACT.Identity),
                 reads=[sc], writes=[att])

        for b in range(NB):
            b0 = b * NTB
            S.dma("sp", Qb[:], self.Q.t[:, :, b0:b0 + NTB], reads=[self.Q], writes=[Qb])
            S.dma("sp", Kb[:], self.K2.t[:, :, b0:b0 + NTB], reads=[self.K2], writes=[Kb])
            S.dma("sp", Vb[:], self.V.t[:, b0 // 128:(b0 + NTB) // 128, :], reads=[self.V], writes=[Vb])
            todo = [(h, blk) for h in range(12) for blk in range(-2, SEQ // 128)]
            active = {}
            ti_ = 0
            while ti_ < len(todo) or active:
                for slot in (0, 1):
                    if slot not in active and ti_ < len(todo):
                        active[slot] = item_gen(todo[ti_][0], todo[ti_][1], slot)
                        ti_ += 1
                        if slot == 1 and len(active) == 2 and ti_ == 2:
                            for _ in range(4):
                                next(active[0])
                for slot in list(active):
                    try:
                        next(active[slot])
                    except StopIteration:
                        del active[slot]
            S.dma("sp", self.MIX.t[:, 2:8, b0:b0 + NTB], att[:], reads=[att], writes=[self.MIX])
        S.barrier()
        self.ps_set = list(range(8))
        st.close()

    def even_pool(self, je):
        S = self.S
        st = ExitStack()
        TM = SEQ
        A = [S.sbuf(f"plA{i}", [128, TM + 16], F32, st) for i in range(5)]
        prod = S.sbuf("plprod", [128, TM], F32, st)
        pooled = S.sbuf("plpooled", [128, TM], BF16, st)
        invx = S.sbuf("plinvx", [128, 2, SEQ], F32, st)
        invc = S.sbuf("plinvc", [128, 2, CTX], F32, st)
        PW = S.sbuf("plW", [128, 2, 128], BF16, st)
        psc = S.sbuf("plsc", [128, 2], F32, st)
        mo = [S.sbuf(f"plmo{i}", [128, 512], BF16, st) for i in range(2)]
        S.dma("sp", invx[:], self.inv_x.t[:, :, :], reads=[self.inv_x], writes=[invx])
        S.dma("sp", invc[:], self.inv_c.t[:, :, :], reads=[self.inv_c], writes=[invc])
        S.dma("pool", PW[:], self.pw_blk.t[je].rearrange("c p n -> p c n"), reads=[self.pw_blk], writes=[PW])
        S.dma("sp", psc[:], self.pscale.t[je], reads=[self.pscale], writes=[psc])
        oi = 0
        for b in range(NB):
            for isc in (True, False):
                T = CTX if isc else SEQ
                t0 = b * NTB + (0 if isc else CTX)
                inv = invc if isc else invx
                for c in range(2):
                    for a in A:
                        S.op("pool", lambda e: e.memset(a[:, 0:8], 0.0), writes=[a])
                        S.op("pool", lambda e: e.memset(a[:, T + 8:T + 16], 0.0), writes=[a])
                    S.dma("sp", A[0][:, 8:T + 8], self.U.t[:, c, t0:t0 + T], reads=[self.U], writes=[A[0]])
                    shifts = [(0, 1), (1, 1), (2, 2), (4, 4)]
                    nlev = 2 if c == 0 else 4
                    for lv in range(nlev):
                        sp_, sm_ = shifts[lv]
                        a_in = A[lv]
                        a_out = A[lv + 1]
                        S.op("dve", lambda e: e.tensor_tensor(a_out[:, 8:T + 8], a_in[:, 8 + sp_:T + 8 + sp_],
                                                              a_in[:, 8 - sm_:T + 8 - sm_], ALU.add),
                             reads=[a_in], writes=[a_out])
                    lo = A[1] if c == 0 else A[3]
                    hi = A[2] if c == 0 else A[4]
                    S.op("dve", lambda e: e.tensor_tensor(prod[0:64, 0:T], lo[0:64, 8:T + 8], inv[0:64, c, 0:T], ALU.mult),
                         reads=[lo, inv], writes=[prod])
                    S.op("dve", lambda e: e.tensor_tensor(prod[64:128, 0:T], hi[64:128, 8:T + 8], inv[64:128, c, 0:T], ALU.mult),
                         reads=[hi, inv], writes=[prod])
                    S.op("pool", lambda e: e.tensor_tensor(pooled[:, 0:T], prod[:, 0:T], A[0][:, 8:T + 8], ALU.subtract),
                         reads=[prod, A[0]], writes=[pooled])
                    for n0 in range(0, T, 512):
                        n = min(512, T - n0)
                        p = self.next_ps()
                        S.op("pe", lambda e: e.matmul(p[:, 0:n], PW[:, c, :], pooled[:, n0:n0 + n], start=True, stop=True),
                             reads=[PW, pooled], writes=[p])
                        m_ = mo[oi % 2]
                        oi += 1
                        S.op("act", lambda e: e.activation(m_[:, 0:n], p[:, 0:n], ACT.Copy, scale=psc[:, c:c + 1]),
                             reads=[p, psc], writes=[m_])
                        S.dma("sp", self.MIX.t[:, c, t0 + n0:t0 + n0 + n], m_[:, 0:n], reads=[m_], writes=[self.MIX])
        S.barrier()
        st.close()

    def mix_out(self, w_ap, wres, src):
        S = self.S
        st = ExitStack()
        Wo = S.sbuf("moW", [128, KC, D], BF16, st)
        for k in range(KC):
            S.dma("pool", Wo[:, k, :], w_ap[k * 128:(k + 1) * 128, :], reads=[wres], writes=[Wo])
        hb = [S.sbuf(f"moh{i}", [128, KC, 512], F32, st) for i in range(2)]
        mb = [S.sbuf(f"mom{i}", [128, KC, 512], BF16, st) for i in range(2)]
        for ti, (t0, n, col, b, isc) in enumerate(token_tiles()):
            h = hb[ti % 2]
            mx = mb[ti % 2]
            S.dma("sp", h[:, :, 0:n], src.t[:, :, t0:t0 + n], reads=[src], writes=[h])
            S.dma("sp", mx[:, :, 0:n], self.MIX.t[:, :, t0:t0 + n], reads=[self.MIX], writes=[mx])
            for m in range(KC):
                p = self.next_ps()
                for k in range(KC):
                    S.op("pe", lambda e: e.matmul(p[:, 0:n], Wo[:, k, m * 128:(m + 1) * 128], mx[:, k, 0:n],
                                                  start=(k == 0), stop=(k == KC - 1)), reads=[Wo, mx], writes=[p])
                S.op("dve", lambda e: e.scalar_tensor_tensor(h[:, m, 0:n], p[:, 0:n], self.mod[:, 16 + m, col:col + 1],
                                                             h[:, m, 0:n], ALU.mult, ALU.add),
                     reads=[p, self.mod, h], writes=[h])
            S.dma("sp", self.Hs.t[:, :, t0:t0 + n], h[:, :, 0:n], reads=[h], writes=[self.Hs])
        S.barrier()
        st.close()

    def even_mixer(self, je, src):
        self.even_proj(je, src)
        self.even_attn(je)
        self.even_pool(je)
        self.mix_out(self.ev_wout.t[je], self.ev_wout, src)

    def odd_proj(self, jo, src):
        S = self.S
        st = ExitStack()
        self.nm_alloc(st)
        W = S.sbuf("odW", [128, KC, 2976], BF16, st)
        for k in range(KC):
            S.dma("pool", W[:, k, :], self.od_w.t[jo, k * 128:(k + 1) * 128, :], reads=[self.od_w], writes=[W])
        mu = S.sbuf("odmu", [128, 22, 2], F32, st)
        S.dma("sp", mu[:], self.mu_l.t[jo], reads=[self.mu_l], writes=[mu])
        c0 = S.sbuf("odc0", [128, 22], F32, st)
        S.op("dve", lambda e: e.tensor_tensor(c0[:], mu[:, :, 0], mu[:, :, 1], ALU.add), reads=[mu], writes=[c0])
        S.op("dve", lambda e: e.tensor_scalar(c0[:], c0[:], -1.0, 1.0, ALU.mult, ALU.add), reads=[c0], writes=[c0])
        ax = S.sbuf("odax", [128, KC, SEQ], BF16, st)
        hb = [S.sbuf(f"odh{i}", [128, KC, 512], F32, st) for i in range(2)]
        ub = [S.sbuf(f"odu{i}", [128, SEQ + 2], F32, st) for i in range(2)]
        t1 = S.sbuf("odt1", [128, SEQ], F32, st)
        fo = [S.sbuf(f"odf{i}", [128, SEQ], BF16, st) for i in range(2)]
        for u in ub:
            S.op("pool", lambda e: e.memset(u[:, 0:1], 0.0), writes=[u])
        tiles = token_tiles()
        ci = 0
        for b in range(NB):
            for isc in (True, False):
                T = CTX if isc else SEQ
                s0 = b * NTB + (0 if isc else CTX)
                segt = [t for t in tiles if t[3] == b and t[4] == isc]
                for ti, (t0, n, col, _, _) in enumerate(segt):
                    h = hb[ti % 2]
                    S.dma("sp", h[:, :, 0:n], src.t[:, :, t0:t0 + n], reads=[src], writes=[h])
                    self.norm_mod_tile(0, h, t0, n, col, ax, ooff=t0 - s0)
                for m in range(24):
                    off = m * 128 if m < 21 else (2688 if m == 21 else 2720 + (m - 22) * 128)
                    mw = 32 if m == 21 else 128
                    u = ub[ci % 2]
                    f = fo[ci % 2]
                    ci += 1
                    for n0 in range(0, T, 512):
                        n = min(512, T - n0)
                        p = self.next_ps()
                        for k in range(KC):
                            S.op("pe", lambda e: e.matmul(p[0:mw, 0:n], W[:, k, off:off + mw], ax[:, k, n0:n0 + n],
                                                          start=(k == 0), stop=(k == KC - 1)), reads=[W, ax], writes=[p])
                        if m >= 22:
                            S.op("act", lambda e: e.activation(f[:, n0:n0 + n], p[:, 0:n], ACT.Identity), reads=[p], writes=[f])
                        else:
                            S.op("act", lambda e: e.activation(u[0:mw, 1 + n0:1 + n0 + n], p[0:mw, 0:n], ACT.Identity),
                                 reads=[p], writes=[u])
                    if m >= 22:
                        S.dma("sp", self.FN.t[:, m - 22, s0:s0 + T], f[:, 0:T], reads=[f], writes=[self.FN])
                        continue
                    S.op("pool", lambda e: e.memset(u[:, T + 1:T + 2], 0.0), writes=[u])
                    S.op("dve", lambda e: e.tensor_scalar(t1[0:mw, 0:T], u[0:mw, 1:T + 1], c0[0:mw, m:m + 1], None, ALU.mult),
                         reads=[u, c0], writes=[t1])
                    S.op("dve", lambda e: e.scalar_tensor_tensor(t1[0:mw, 0:T], u[0:mw, 0:T], mu[0:mw, m, 0:1], t1[0:mw, 0:T],
                                                                 ALU.mult, ALU.add), reads=[u, mu, t1], writes=[t1])
                    S.op("dve", lambda e: e.scalar_tensor_tensor(f[0:mw, 0:T], u[0:mw, 2:T + 2], mu[0:mw, m, 1:2], t1[0:mw, 0:T],
                                                                 ALU.mult, ALU.add), reads=[u, mu, t1], writes=[f])
                    S.dma("sp", self.F.t[0:mw, m, s0:s0 + T], f[0:mw, 0:T], reads=[f], writes=[self.F])
        S.barrier()
        st.close()

    def odd_rwkv(self, jo):
        S = self.S
        st = ExitStack()
        N = NTB
        NTL = N // 128
        CE = float(np.exp(-0.5))
        sb = lambda nm, shp, dt: S.sbuf(nm, shp, dt, st)
        idb = sb("rw_idb", [128, 128], BF16)
        S.op("dve", lambda e: e.tensor_copy(idb[:], self.ident[:]), reads=[self.ident], writes=[idb])
        bo_bf = sb("rw_bo", [128, 128], BF16)
        bo_f = sb("rw_bof", [128, 128], F32)
        S.dma("sp", bo_f[:], self.blk1.t[:, :], reads=[self.blk1], writes=[bo_f])
        S.op("dve", lambda e: e.tensor_copy(bo_bf[:], bo_f[:]), reads=[bo_f], writes=[bo_bf])
        bo64 = sb("rw_bo64", [128, 128], F32)
        S.op("dve", lambda e: e.tensor_scalar(bo64[:], bo_f[:], 1.0 / 64, None, ALU.mult), reads=[bo_f], writes=[bo64])
        msk = sb("rw_msk", [128, 4, 128], F32)
        S.dma("sp", msk[:], self.rmask.t.rearrange("v p n -> p v n"), reads=[self.rmask], writes=[msk])
        nmsk = sb("rw_nmsk", [128, 4, 128], F32)
        S.op("dve", lambda e: e.tensor_scalar(nmsk[:], msk[:], -1.0, None, ALU.mult), reads=[msk], writes=[nmsk])
        w2s = sb("rw_w2", [128, 768], BF16)
        a2s = sb("rw_a2", [128, 768], BF16)
        g2a = sb("rw_g2a", [128, 768], BF16)
        g2b = sb("rw_g2b", [32, 768], BF16)
        S.dma("pool", w2s[:], self.rw_w2.t[jo], reads=[self.rw_w2], writes=[w2s])
        S.dma("pool", a2s[:], self.rw_a2.t[jo], reads=[self.rw_a2], writes=[a2s])
        S.dma("pool", g2a[:], self.rw_g2.t[jo, 0:128, :], reads=[self.rw_g2], writes=[g2a])
        S.dma("pool", g2b[:], self.rw_g2.t[jo, 128:160, :], reads=[self.rw_g2], writes=[g2b])
        pv = sb("rw_pv", [128, 6, 10], F32)
        S.dma("sp", pv[:, :, 0:9], self.rw_pv.t[jo], reads=[self.rw_pv], writes=[pv])
        S.op("dve", lambda e: e.tensor_scalar(pv[:, :, 9], pv[:, :, 5], -1.0, 1.0, ALU.mult, ALU.add), reads=[pv], writes=[pv])
        ones = sb("rw_ones", [128, N], BF16)
        S.op("pool", lambda e: e.memset(ones[:], 1.0), writes=[ones])
        r_f = sb("rw_r", [128, N], BF16)
        k_f = sb("rw_k", [128, N], BF16)
        v_f = sb("rw_v", [128, N], BF16)
        dlc = sb("rw_dl", [128, N], BF16)
        alc = sb("rw_al", [128, N], BF16)
        gl0 = sb("rw_gl0", [128, N], BF16)
        gl1 = sb("rw_gl1", [32, N], BF16)
        th = dlc
        kk = sb("rw_kk", [128, N], BF16)
        sg = sb("rw_sg", [128, N], F32)
        A_ = sb("rw_A", [128, N], F32)
        Asum = sb("rw_Asum", [128, N], F32)
        cum = sb("rw_cum", [128, N], F32)
        kd = sb("rw_kd", [128, N], BF16)
        bq = sb("rw_bq", [128, N], BF16)
        rt = sb("rw_rt", [128, N], BF16)
        P_f = sb("rw_Pf", [128, N], BF16)
        Q_t = sb("rw_Qt", [128, NTL, 2, 64], BF16)
        Kb_t = sb("rw_Kbt", [128, NTL, 128], BF16)
        NBb_t = sb("rw_NBbt", [128, NTL, 128], BF16)
        V_t = sb("rw_Vt", [128, NTL, 128], BF16)
        ArkT = [sb(f"rw_ArkT{i}", [128, NTL, 128], BF16) for i in range(2)]
        NArbT = [sb(f"rw_NArbT{i}", [128, NTL, 128], BF16) for i in range(2)]
        WL = sb("rw_WL", [128, 2 * NTL], F32)
        Yacc = sb("rw_Y", [128, N], F32)
        Mst = sb("rw_M", [128, 64], F32)
        Mb_all = sb("rw_Mball", [128, 2 * NTL, 64], BF16)
        GpT = sb("rw_GpT", [128, 2 * NTL, 64], BF16)
        Hs_ = sb("rw_Hs", [128, 2 * NTL, 64], F32)
        T1 = [[sb(f"rw_T1{i}_{j}", [128, 64], F32) for j in range(2)] for i in range(2)]
        Mstv = [Res("Mst0", Mst.t), Res("Mst1", Mst.t)]
        Mbv = [Res("Mb0", Mb_all.t), Res("Mb1", Mb_all.t)]
        GpTv = [Res("GpT0", GpT.t), Res("GpT1", GpT.t)]
        Hsv = [Res("Hs0", Hs_.t), Res("Hs1", Hs_.t)]
        Yv = [Res("Y0", Yacc.t), Res("Y1", Yacc.t)]
        PT = [[sb(f"rw_PT{i}_{j}", [128, 64], BF16) for j in range(2)] for i in range(2)]
        Qv = [Res("Q0", Q_t.t), Res("Q1", Q_t.t)]
        E = [sb(f"rw_E{i}", [128, 128], F32) for i in range(2)]
        Ep = [sb(f"rw_Ep{i}", [128, 128], F32) for i in range(2)]
        ex = [[sb(f"rw_ex{i}_{j}", [128, 128], F32) for j in range(4)] for i in range(2)]
        kt = [sb(f"rw_kt{i}", [128, 128], BF16) for i in range(2)]
        bt = [sb(f"rw_bt{i}", [128, 128], BF16) for i in range(2)]
        kkh = [sb(f"rw_kkh{i}", [128, 128], BF16) for i in range(2)]
        kbar = [sb(f"rw_kbar{i}", [128, 128], BF16) for i in range(2)]
        nbbar = [sb(f"rw_nbbar{i}", [128, 128], BF16) for i in range(2)]
        KKt = [sb(f"rw_KKt{i}", [128, 128], BF16) for i in range(2)]
        mats = [[[sb(f"rw_mat{a}_{i}_{j}", [128, 128], BF16) for j in range(8)] for i in range(2)] for a in range(2)]
        AVt = [[sb(f"rw_AVt{a}_{i}", [128, 64], BF16) for i in range(2)] for a in range(2)]
        big = [sb(f"rw_big{i}", [128, 512], F32) for i in range(4)]
        bigb = [sb(f"rw_bigb{i}", [128, 512], BF16) for i in range(2)]
        sgl0 = gl0
        sgl1 = gl1
        mo = [sb(f"rw_mo{i}", [128, 512], BF16) for i in range(2)]
        evi = [0]

        def evac(dst_ap, dst_res, src_ap, src_res):
            evi[0] += 1
            if evi[0] % 4 != 0:
                S.op("act", lambda e: e.activation(dst_ap, src_ap, ACT.Identity), reads=[src_res], writes=[dst_res])
            else:
                S.op("dve", lambda e: e.tensor_copy(dst_ap, src_ap), reads=[src_res], writes=[dst_res])

        def mm(p, pap, lhsT, lres, rhs, rres, start=True, stop=True):
            S.op("pe", lambda e: e.matmul(pap, lhsT, rhs, start=start, stop=stop), reads=[lres, rres], writes=[p])

        stage = self.dbg.get("rw_stage", 9)

        class _Stop(Exception):
            pass
        try:
          self._rwkv_body(locals(), _Stop)
        except _Stop:
            pass
        S.barrier()
        st.close()

    def _rwkv_body(self, L, _Stop):
        globals_ = L
        S = self.S
        (N, NTL, CE, idb, bo_bf, bo_f, bo64, msk, nmsk, w2s, a2s, g2a, g2b, pv, ones, r_f, k_f, v_f, dlc, alc, gl0, gl1, th, kk, sg,
         A_, Asum, cum, kd, bq, rt, P_f, Q_t, Kb_t, NBb_t, V_t, ArkT, NArbT, WL, Yacc, Mst, Mb_all, GpT, Hs_, T1, Mstv, Mbv, GpTv, Hsv, Yv, PT, Qv,
         E, Ep, ex, kt, bt, kkh, kbar,
         nbbar, KKt, mats, AVt, big, bigb, sgl0, sgl1, mo, evac, mm, stage) = [L[k] for k in (
            "N NTL CE idb bo_bf bo_f bo64 msk nmsk w2s a2s g2a g2b pv ones r_f k_f v_f dlc alc gl0 gl1 th kk sg "
            "A_ Asum cum kd bq rt P_f Q_t Kb_t NBb_t V_t ArkT NArbT WL Yacc Mst Mb_all GpT Hs_ T1 Mstv Mbv GpTv Hsv Yv PT Qv E Ep ex kt bt kkh kbar "
            "nbbar KKt mats AVt big bigb sgl0 sgl1 mo evac mm stage").split()]
        for b in range(NB):
            b0 = b * NTB
            S.dma("sp", dlc[:], self.F.t[:, 18, b0:b0 + N], reads=[self.F], writes=[dlc])
            S.dma("sp", alc[:], self.F.t[:, 19, b0:b0 + N], reads=[self.F], writes=[alc])
            S.dma("sp", gl0[:], self.F.t[:, 20, b0:b0 + N], reads=[self.F], writes=[gl0])
            S.dma("sp", gl1[:], self.F.t[0:32, 21, b0:b0 + N], reads=[self.F], writes=[gl1])
            S.op("act", lambda e: e.activation(th[:], dlc[:], ACT.Tanh), reads=[dlc], writes=[dlc])
            S.op("act", lambda e: e.activation(sgl0[:], gl0[:], ACT.Sigmoid), reads=[gl0], writes=[gl0])
            S.op("act", lambda e: e.activation(sgl1[:], gl1[:], ACT.Sigmoid), reads=[gl1], writes=[gl1])
            for hp in range(6):
                S.dma("sp", r_f[:], self.F.t[:, hp, b0:b0 + N], reads=[self.F], writes=[r_f])
                S.dma("sp", k_f[:], self.F.t[:, 6 + hp, b0:b0 + N], reads=[self.F], writes=[k_f])
                S.dma("sp", v_f[:], self.F.t[:, 12 + hp, b0:b0 + N], reads=[self.F], writes=[v_f])
                S.op("dve", lambda e: e.tensor_scalar(kk[:], k_f[:], pv[:, hp, 4:5], None, ALU.mult), reads=[k_f, pv], writes=[kk])
                for n0 in range(0, N, 512):
                    n = min(512, N - n0)
                    sq = bigb[(n0 // 512) % 2]
                    S.op("act", lambda e: e.activation(sq[:, 0:n], kk[:, n0:n0 + n], ACT.Square), reads=[kk], writes=[sq])
                    p = self.next_ps()
                    mm(p, p[:, 0:n], bo_bf[:], bo_bf, sq[:, 0:n], sq)
                    nr = big[(n0 // 512) % 2]
                    S.op("act", lambda e: e.activation(nr[:, 0:n], p[:, 0:n], ACT.Sqrt), reads=[p], writes=[nr])
                    S.op("dve", lambda e: e.tensor_scalar(nr[:, 0:n], nr[:, 0:n], 1e-12, None, ALU.max), reads=[nr], writes=[nr])
                    S.op("dve", lambda e: e.reciprocal(nr[:, 0:n], nr[:, 0:n]), reads=[nr], writes=[nr])
                    S.op("dve", lambda e: e.tensor_tensor(kk[:, n0:n0 + n], kk[:, n0:n0 + n], nr[:, 0:n], ALU.mult),
                         reads=[kk, nr], writes=[kk])
                if stage <= 0:
                    raise _Stop()
                for tl in range(NTL):
                    p = self.next_ps()
                    mm(p, p[:, 0:128], v_f[:, tl * 128:(tl + 1) * 128], v_f, idb[:], idb)
                    evac(V_t[:, tl, :], V_t, p[:, 0:128], p)
                for d in range(2):
                    ds = slice(d * 64, (d + 1) * 64)
                    for n0 in range(0, N, 512):
                        n = min(512, N - n0)
                        p = self.next_ps()
                        mm(p, p[:, 0:n], w2s[ds, hp * 128:(hp + 1) * 128], w2s, th[ds, n0:n0 + n], th)
                        S.op("act", lambda e: e.activation(sg[:, n0:n0 + n], p[:, 0:n], ACT.Sigmoid, bias=pv[:, hp, d:d + 1]),
                             reads=[p, pv], writes=[sg])
                        p = self.next_ps()
                        mm(p, p[:, 0:n], a2s[ds, hp * 128:(hp + 1) * 128], a2s, alc[ds, n0:n0 + n], alc)
                        S.op("act", lambda e: e.activation(A_[:, n0:n0 + n], p[:, 0:n], ACT.Sigmoid, bias=pv[:, hp, 2 + d:3 + d]),
                             reads=[p, pv], writes=[A_])
                    if d == 0:
                        S.op("pool", lambda e: e.tensor_copy(Asum[:], A_[:]), reads=[A_], writes=[Asum])
                    else:
                        S.op("pool", lambda e: e.tensor_tensor(Asum[:], Asum[:], A_[:], ALU.add), reads=[A_, Asum], writes=[Asum])
                    S.op("dve", lambda e: e.tensor_tensor_scan(cum[:], ones[:], sg[:], 0.0, ALU.mult, ALU.add),
                         reads=[ones, sg], writes=[cum])
                    S.op("dve", lambda e: e.tensor_scalar(kd[:], A_[:], pv[:, hp, 5:6], pv[:, hp, 9:10], ALU.mult, ALU.add),
                         reads=[A_, pv], writes=[kd])
                    S.op("dve", lambda e: e.tensor_tensor(kd[:], kd[:], k_f[:], ALU.mult), reads=[kd, k_f], writes=[kd])
                    S.op("pool", lambda e: e.tensor_tensor(bq[:], kk[:], A_[:], ALU.mult), reads=[kk, A_], writes=[bq])
                    if stage <= 1:
                        raise _Stop()
                    ms, mi = (0, 1) if d == 0 else (2, 3)
                    msT = 2 if d == 0 else 0
                    pending = []
                    for tl in range(NTL):
                        i2 = tl % 2
                        ts_ = slice(tl * 128, (tl + 1) * 128)
                        e_, ep_ = E[i2], Ep[i2]
                        eW, eWi, eWp, eWl = ex[i2]
                        for half in range(2):
                            cc = 2 * tl + half
                            c_ = slice(cc * 64, (cc + 1) * 64)
                            l_ = slice(half * 64, (half + 1) * 64)
                            if d == 0:
                                if cc == 0:
                                    S.op("dve", lambda e: e.tensor_scalar(e_[:, l_], cum[:, c_], -CE, None, ALU.mult),
                                         reads=[cum], writes=[e_])
                                else:
                                    S.op("dve", lambda e: e.tensor_scalar(e_[:, l_], cum[:, c_], cum[:, cc * 64 - 1:cc * 64], -CE,
                                                                          ALU.subtract, ALU.mult), reads=[cum], writes=[e_])
                                S.op("dve", lambda e: e.scalar_tensor_tensor(ep_[:, l_], sg[:, c_], CE, e_[:, l_], ALU.mult, ALU.add),
                                     reads=[sg, e_], writes=[ep_])
                                tot = e_[:, half * 64 + 63:half * 64 + 64]
                            else:
                                S.op("dve", lambda e: e.tensor_scalar(ep_[:, l_], cum[:, c_], cum[:, cc * 64 + 63:cc * 64 + 64], CE,
                                                                      ALU.subtract, ALU.mult), reads=[cum], writes=[ep_])
                                S.op("dve", lambda e: e.scalar_tensor_tensor(e_[:, l_], sg[:, c_], -CE, ep_[:, l_], ALU.mult, ALU.add),
                                     reads=[sg, ep_], writes=[e_])
                                tot = e_[:, half * 64:half * 64 + 1]
                            S.op("act", lambda e: e.activation(eWl[:, l_], e_[:, l_], ACT.Exp, bias=tot, scale=-1.0),
                                 reads=[e_], writes=[eWl])
                            S.op("act", lambda e: e.activation(WL[:, cc:cc + 1], tot, ACT.Exp), reads=[e_], writes=[WL])
                        S.op("act", lambda e: e.activation(eW[:], e_[:], ACT.Exp), reads=[e_], writes=[eW])
                        S.op("act", lambda e: e.activation(eWi[:], e_[:], ACT.Exp, scale=-1.0), reads=[e_], writes=[eWi])
                        S.op("act", lambda e: e.activation(eWp[:], ep_[:], ACT.Exp), reads=[ep_], writes=[eWp])
                        S.op("dve", lambda e: e.tensor_tensor(rt[:, ts_], r_f[:, ts_], eW[:], ALU.mult), reads=[r_f, eW], writes=[rt])
                        S.op("pool", lambda e: e.tensor_tensor(kt[i2][:], kd[:, ts_], eWi[:], ALU.mult), reads=[kd, eWi], writes=[kt[i2]])
                        S.op("dve", lambda e: e.tensor_tensor(bt[i2][:], bq[:, ts_], eWi[:], ALU.mult), reads=[bq, eWi], writes=[bt[i2]])
                        S.op("pool", lambda e: e.tensor_tensor(kkh[i2][:], kk[:, ts_], eWp[:], ALU.mult), reads=[kk, eWp], writes=[kkh[i2]])
                        S.op("dve", lambda e: e.tensor_tensor(kbar[i2][:], kd[:, ts_], eWl[:], ALU.mult), reads=[kd, eWl], writes=[kbar[i2]])
                        S.op("dve", lambda e: e.scalar_tensor_tensor(nbbar[i2][:], bq[:, ts_], -1.0, eWl[:], ALU.mult, ALU.mult),
                             reads=[bq, eWl], writes=[nbbar[i2]])
                        if stage == 2 and self.dbg.get("rw_sub", 9) <= 0:
                            continue
                        for (src_, dst_ap, dst_r) in ((kkh[i2], KKt[i2][:], KKt[i2]), (kbar[i2], Kb_t[:, tl, :], Kb_t),
                                                      (nbbar[i2], NBb_t[:, tl, :], NBb_t)):
                            p = self.next_ps()
                            mm(p, p[:, 0:128], src_[:], src_, idb[:], idb)
                            evac(dst_ap, dst_r, p[:, 0:128], p)
                        def head_gen(tl=tl, i2=i2, ts_=ts_):
                            def one(hh):
                                hs = slice(hh * 64, (hh + 1) * 64)
                                X, XT, Y, YT, R, AkkT_, Y2, YT2 = mats[i2][hh]

                                def masked(dst_ap, dst_r, lhs, rhs_ap, rhs_r, mk, neg=False):
                                    p = self.next_ps()
                                    mm(p, p[:, 0:128], lhs[hs, :], lhs, rhs_ap, rhs_r)
                                    mres = nmsk if neg else msk
                                    S.op("dve", lambda e: e.tensor_tensor(dst_ap, p[:, 0:128], mres[:, mk, :], ALU.mult),
                                         reads=[p, mres], writes=[dst_r])
                                masked(X[:], X, bt[i2], kkh[i2][hs, :], kkh[i2], ms)
                                masked(XT[:], XT, kkh[i2], bt[i2][hs, :], bt[i2], msT)
                                yield
                                masked(AkkT_[:], AkkT_, kt[i2], kkh[i2][hs, :], kkh[i2], ms)
                                masked(ArkT[hh][:, tl, :], ArkT[hh], kt[i2], rt[hs, ts_], rt, mi)
                                masked(NArbT[hh][:, tl, :], NArbT[hh], bt[i2], rt[hs, ts_], rt, mi, neg=True)
                                S.op("dve", lambda e: e.tensor_tensor(R[:], idb[:], X[:], ALU.subtract), reads=[idb, X], writes=[R])
                                p = self.next_ps()
                                mm(p, p[:, 0:64], AkkT_[:], AkkT_, V_t[:, tl, hs], V_t)
                                evac(AVt[i2][hh][:], AVt[i2][hh], p[:, 0:64], p)
                                yield
                                cy, cyt, ny, nyt = X, XT, Y, YT
                                for lvl in range(5):
                                    if lvl < 4:
                                        p = self.next_ps()
                                        mm(p, p[:, 0:128], cyt[:], cyt, cy[:], cy)
                                        evac(ny[:], ny, p[:, 0:128], p)
                                    p = self.next_ps()
                                    mm(p, p[:, 0:128], cy[:], cy, cyt[:], cyt)
                                    evac(nyt[:], nyt, p[:, 0:128], p)
                                    yield
                                    p = self.next_ps()
                                    mm(p, p[:, 0:128], nyt[:], nyt, R[:], R)
                                    S.op("dve", lambda e: e.tensor_tensor(R[:], p[:, 0:128], R[:], ALU.add), reads=[p, R], writes=[R])
                                    cy, cyt = ny, nyt
                                    ny, nyt = (Y2, YT2) if ny is Y else (Y, YT)
                                    yield
                                p = self.next_ps()
                                mm(p, p[hs, 0:128], KKt[i2][:, hs], KKt[i2], R[:], R)
                                evac(P_f[hs, ts_], P_f, p[hs, 0:128], p)
                                p = self.next_ps()
                                mm(p, p[:, 0:64], R[:], R, KKt[i2][:, hs], KKt[i2])
                                evac(PT[i2][hh][:], PT[i2][hh], p[:, 0:64], p)
                                p = self.next_ps()
                                mm(p, p[:, 0:64], R[:], R, AVt[i2][hh][:], AVt[i2][hh])
                                evac(Q_t[:, tl, hh, :], Qv[hh], p[:, 0:64], p)
                                yield
                                for half in range(2):
                                    cc = 2 * tl + half
                                    cs = slice(half * 64, (half + 1) * 64)
                                    pg = self.next_ps()
                                    mm(pg, pg[hs, 0:64], PT[i2][hh][cs, :], PT[i2][hh], NBb_t[cs, tl, hs], NBb_t)
                                    evac(GpT[hs, cc, :], GpTv[hh], pg[hs, 0:64], pg)
                                    ph = self.next_ps()
                                    mm(ph, ph[hs, 0:64], Kb_t[cs, tl, hs], Kb_t, V_t[cs, tl, hs], V_t, start=True, stop=False)
                                    mm(ph, ph[hs, 0:64], NBb_t[cs, tl, hs], NBb_t, Q_t[cs, tl, hh, :], Qv[hh], start=False, stop=True)
                                    evac(Hs_[hs, cc, :], Hsv[hh], ph[hs, 0:64], ph)
                            return [one(0), one(1)]
                        pending.extend(head_gen())
                        if tl % 2 == 1 or tl == NTL - 1:
                            while pending:
                                for g_ in list(pending):
                                    try:
                                        next(g_)
                                    except StopIteration:
                                        pending.remove(g_)
                    if stage <= 2:
                        raise _Stop()
                    if d == 0:
                        order = list(range(2 * NTL))
                    else:
                        order = list(range(CTX // 64 - 1, -1, -1)) + list(range(2 * NTL - 1, CTX // 64 - 1, -1))
                    S.op("pool", lambda e: e.memset(Mst[:], 0.0), writes=[Mstv[0], Mstv[1]])
                    S.op("pool", lambda e: e.memset(Mb_all[:, order[0], :], 0.0), writes=[Mbv[0], Mbv[1]])
                    def chain_step(oi):
                        cc = order[oi]
                        ncc = order[oi + 1] if oi + 1 < len(order) else None
                        for hh in range(2):
                            hs = slice(hh * 64, (hh + 1) * 64)
                            t1 = T1[hh][oi % 2]
                            S.op("dve", lambda e: e.scalar_tensor_tensor(t1[hs, :], Mst[hs, :], WL[hs, cc:cc + 1], Hs_[hs, cc, :],
                                                                         ALU.mult, ALU.add),
                                 reads=[Mstv[hh], WL, Hsv[hh]], writes=[t1])
                            p = self.next_ps()
                            mm(p, p[hs, 0:64], GpT[hs, cc, :], GpTv[hh], Mb_all[hs, cc, :], Mbv[hh])
                            if ncc is not None:
                                S.op("dve", lambda e: e.tensor_tensor(Mb_all[hs, ncc, :], p[hs, 0:64], t1[hs, :], ALU.add),
                                     reads=[p, t1], writes=[Mbv[hh]])
                                S.op("dve", lambda e: e.tensor_tensor(Mst[hs, :], p[hs, 0:64], t1[hs, :], ALU.add),
                                     reads=[p, t1], writes=[Mstv[hh]])

                    def bulk_u(cc):
                        tl, half = cc // 2, cc % 2
                        cs = slice(half * 64, (half + 1) * 64)
                        c_ = slice(cc * 64, (cc + 1) * 64)
                        for hh in range(2):
                            hs = slice(hh * 64, (hh + 1) * 64)
                            pu = self.next_ps()
                            mm(pu, pu[cs, 0:64], P_f[hs, c_], P_f, Mb_all[hs, cc, :], Mbv[hh])
                            S.op("dve", lambda e: e.tensor_tensor(Q_t[cs, tl, hh, :], pu[cs, 0:64], Q_t[cs, tl, hh, :], ALU.add),
                                 reads=[pu, Qv[hh]], writes=[Qv[hh]])

                    def bulk_y(cc):
                        tl, half = cc // 2, cc % 2
                        cs = slice(half * 64, (half + 1) * 64)
                        c_ = slice(cc * 64, (cc + 1) * 64)
                        for hh in range(2):
                            hs = slice(hh * 64, (hh + 1) * 64)
                            py = self.next_ps()
                            mm(py, py[hs, 0:64], Mb_all[hs, cc, :], Mbv[hh], rt[hs, c_], rt, start=True, stop=True)
                            py2 = self.next_ps()
                            mm(py2, py2[hs, 0:64], V_t[cs, tl, hs], V_t, ArkT[hh][cs, tl, cs], ArkT[hh], start=True, stop=False)
                            mm(py2, py2[hs, 0:64], Q_t[cs, tl, hh, :], Qv[hh], NArbT[hh][cs, tl, cs], NArbT[hh], start=False, stop=True)
                            if d == 0:
                                S.op("act", lambda e: e.activation(Yacc[hs, c_], py[hs, 0:64], ACT.Identity), reads=[py], writes=[Yv[hh]])
                            else:
                                S.op("dve", lambda e: e.tensor_tensor(Yacc[hs, c_], py[hs, 0:64], Yacc[hs, c_], ALU.add),
                                     reads=[py, Yv[hh]], writes=[Yv[hh]])
                            S.op("dve", lambda e: e.tensor_tensor(Yacc[hs, c_], py2[hs, 0:64], Yacc[hs, c_], ALU.add),
                                 reads=[py2, Yv[hh]], writes=[Yv[hh]])

                    nord = len(order)
                    for oi in range(nord + 2):
                        if oi < nord:
                            chain_step(oi)
                        if 1 <= oi <= nord:
                            bulk_u(order[oi - 1])
                        if oi >= 2:
                            bulk_y(order[oi - 2])
                if stage <= 3:
                    raise _Stop()
                for n0 in range(0, N, 512):
                    n = min(512, N - n0)
                    ns = slice(n0, n0 + n)
                    yc, sq_, yn, cf = big
                    p = self.next_ps()
                    mm(p, p[:, 0:n], bo64[:], bo64, Yacc[:, ns], Yv[0])
                    S.op("dve", lambda e: e.tensor_tensor(yc[:, 0:n], Yacc[:, ns], p[:, 0:n], ALU.subtract), reads=[Yv[0], Yv[1], p], writes=[yc])
                    S.op("act", lambda e: e.activation(sq_[:, 0:n], yc[:, 0:n], ACT.Square), reads=[yc], writes=[sq_])
                    p = self.next_ps()
                    mm(p, p[:, 0:n], bo64[:], bo64, sq_[:, 0:n], sq_)
                    S.op("act", lambda e: e.activation(sq_[:, 0:n], p[:, 0:n], ACT.Sqrt, bias=self.gneps[:, 0:1]), reads=[p, self.gneps], writes=[sq_])
                    S.op("dve", lambda e: e.reciprocal(sq_[:, 0:n], sq_[:, 0:n]), reads=[sq_], writes=[sq_])
                    S.op("dve", lambda e: e.tensor_tensor(yn[:, 0:n], yc[:, 0:n], sq_[:, 0:n], ALU.mult), reads=[yc, sq_], writes=[yn])
                    S.op("dve", lambda e: e.tensor_scalar(yn[:, 0:n], yn[:, 0:n], pv[:, hp, 7:8], pv[:, hp, 8:9], ALU.mult, ALU.add),
                         reads=[yn, pv], writes=[yn])
                    S.op("dve", lambda e: e.tensor_scalar(cf[:, 0:n], Asum[:, ns], pv[:, hp, 5:6], pv[:, hp, 9:10], ALU.mult, ALU.add),
                         reads=[Asum, pv], writes=[cf])
                    S.op("dve", lambda e: e.tensor_scalar(cf[:, 0:n], cf[:, 0:n], pv[:, hp, 9:10], None, ALU.add),
                         reads=[cf, pv], writes=[cf])
                    S.op("dve", lambda e: e.tensor_tensor(cf[:, 0:n], cf[:, 0:n], k_f[:, ns], ALU.mult), reads=[cf, k_f], writes=[cf])
                    pb = bigb[0]
                    S.op("dve", lambda e: e.scalar_tensor_tensor(pb[:, 0:n], cf[:, 0:n], pv[:, hp, 6:7], r_f[:, ns], ALU.mult, ALU.mult),
                         reads=[cf, pv, r_f], writes=[pb])
                    p = self.next_ps()
                    mm(p, p[:, 0:n], bo_bf[:], bo_bf, pb[:, 0:n], pb)
                    S.op("dve", lambda e: e.tensor_tensor(cf[:, 0:n], p[:, 0:n], v_f[:, ns], ALU.mult), reads=[p, v_f], writes=[cf])
                    S.op("pool", lambda e: e.tensor_tensor(yn[:, 0:n], yn[:, 0:n], cf[:, 0:n], ALU.add), reads=[yn, cf], writes=[yn])
                    p = self.next_ps()
                    mm(p, p[:, 0:n], g2a[:, hp * 128:(hp + 1) * 128], g2a, sgl0[:, ns], sgl0, start=True, stop=False)
                    mm(p, p[:, 0:n], g2b[:, hp * 128:(hp + 1) * 128], g2b, sgl1[:, ns], sgl1, start=False, stop=True)
                    m_ = mo[(n0 // 512) % 2]
                    S.op("dve", lambda e: e.tensor_tensor(m_[:, 0:n], p[:, 0:n], yn[:, 0:n], ALU.mult), reads=[p, yn], writes=[m_])
                    S.dma("sp", self.MIX.t[:, hp, b0 + n0:b0 + n0 + n], m_[:, 0:n], reads=[m_], writes=[self.MIX])

    def odd_fnet(self, jo):
        S = self.S
        st = ExitStack()
        CB = S.sbuf("fnCB", [128, 128], BF16, st)
        SB = S.sbuf("fnSB", [128, 128], BF16, st)
        S.dma("sp", CB[:], self.dft64.t[0], reads=[self.dft64], writes=[CB])
        S.dma("sp", SB[:], self.dft64.t[1], reads=[self.dft64], writes=[SB])
        CT = S.sbuf("fnCT", [128, SEQ // 128, SEQ], BF16, st)
        NST = S.sbuf("fnNST", [128, SEQ // 128, SEQ], BF16, st)
        u = S.sbuf("fnu", [128, 2, SEQ], BF16, st)
        AC = S.sbuf("fnAC", [128, SEQ // 128, 2, 128], BF16, st)
        AS = S.sbuf("fnAS", [128, SEQ // 128, 2, 128], BF16, st)
        mo = [S.sbuf(f"fnmo{i}", [128, 512], BF16, st) for i in range(2)]
        oi = 0
        for isc in (True, False):
            T = CTX if isc else SEQ
            NTT = T // 128
            tab = self.dftc if isc else self.dftx
            for tl in range(NTT):
                S.dma("sp", CT[:, tl, 0:T], tab.t[0, :, tl, :], reads=[tab], writes=[CT])
                S.dma("sp", NST[:, tl, 0:T], tab.t[1, :, tl, :], reads=[tab], writes=[NST])
            for b in range(NB):
                s0 = b * NTB + (0 if isc else CTX)
                S.dma("sp", u[:, :, 0:T], self.FN.t[:, :, s0:s0 + T], reads=[self.FN], writes=[u])
                for tl in range(NTT):
                    for ch in range(2):
                        for (dst, tb) in ((AC, CB), (AS, SB)):
                            p = self.next_ps()
                            S.op("pe", lambda e: e.matmul(p[:, 0:128], u[:, ch, tl * 128:(tl + 1) * 128], tb[:], start=True, stop=True),
                                 reads=[u, tb], writes=[p])
                            S.op("act" if dst is AC else "dve",
                                 (lambda e: e.activation(dst[:, tl, ch, :], p[:, 0:128], ACT.Identity)) if dst is AC else
                                 (lambda e: e.tensor_copy(dst[:, tl, ch, :], p[:, 0:128])), reads=[p], writes=[dst])
                for ch in range(2):
                    for n0 in range(0, T, 512):
                        n = min(512, T - n0)
                        p = self.next_ps()
                        for tl in range(NTT):
                            S.op("pe", lambda e: e.matmul(p[:, 0:n], AC[:, tl, ch, :], CT[:, tl, n0:n0 + n], start=(tl == 0), stop=False),
                                 reads=[AC, CT], writes=[p])
                            S.op("pe", lambda e: e.matmul(p[:, 0:n], AS[:, tl, ch, :], NST[:, tl, n0:n0 + n], start=False,
                                                          stop=(tl == NTT - 1)), reads=[AS, NST], writes=[p])
                        m_ = mo[oi % 2]
                        oi += 1
                        S.op("act", lambda e: e.activation(m_[:, 0:n], p[:, 0:n], ACT.Identity), reads=[p], writes=[m_])
                        S.dma("sp", self.MIX.t[:, 6 + ch, s0 + n0:s0 + n0 + n], m_[:, 0:n], reads=[m_], writes=[self.MIX])
        S.barrier()
        st.close()

    def odd_mixer(self, jo, src):
        parts = self.dbg.get("odd", ("proj", "rwkv", "fnet", "out"))
        if "proj" in parts:
            self.odd_proj(jo, src)
        if "rwkv" in parts:
            self.odd_rwkv(jo)
        if "fnet" in parts:
            self.odd_fnet(jo)
        if "out" in parts:
            self.mix_out(self.od_wout.t[jo], self.od_wout, src)

    def moe(self, li, src, groups=None):
        S = self.S
        NG = 1536
        if groups is None:
            groups = [(g * NG, NG) for g in range(NT // NG)]
        st = ExitStack()
        fx = S.sbuf("moe_fx", [128, KC, NG], BF16, st)
        acc = S.sbuf("moe_acc", [128, KC, NG], F32, st)
        hid = S.sbuf("moe_hid", [128, KC, NG], BF16, st)
        gbc = [S.sbuf(f"moe_gbc{i}", [128, NG], BF16, st) for i in range(2)]
        wgu = [S.sbuf(f"moe_wgu{i}", [128, KC, 256], BF16, st) for i in range(3)]
        wst = [S.sbuf(f"moe_wst{i}", [128, KC, 256], F32, st) for i in range(2)]
        si = 0
        wdn = [S.sbuf(f"moe_wdn{i}", [128, KC, 256], BF16, st) for i in range(3)]
        bgu = S.sbuf("moe_bgu", [128, NE, 16], F32, st)
        bdn = S.sbuf("moe_bdn", [NE, D], F32, st)
        sel = S.sbuf("moe_sel", [NE, NE * 128], F32, st)
        t_g = [S.sbuf(f"moe_tg{i}", [128, 512], F32, st) for i in range(2)]
        t_s = [S.sbuf(f"moe_ts{i}", [128, 512], F32, st) for i in range(2)]
        t_l = [S.sbuf(f"moe_tl{i}", [128, 512], F32, st) for i in range(2)]
        hb = [S.sbuf(f"moe_h{i}", [128, KC, 128], F32, st) for i in range(2)]
        S.dma("sp", bgu[:], self.b_gu.t[li], reads=[self.b_gu], writes=[bgu])
        S.dma("sp", bdn[:], self.b_dn.t[li], reads=[self.b_dn], writes=[bdn])
        S.dma("sp", sel[:], self.sel.t[:, :], reads=[self.sel], writes=[sel])
        wi = 0
        di = 0
        ev = 0
        dbg = self.dbg
        for (g0, ng) in groups[:dbg.get("ngrp", len(groups))]:
            ntg = ng // 512
            S.dma("sp", fx[:, :, 0:ng], self.FX.t[:, :, g0:g0 + ng], reads=[self.FX], writes=[fx])
            for e_ in range(dbg.get("ne", NE)):
                gb = gbc[e_ % 2]
                for tg in range(ntg if dbg.get("gate", True) else 0):
                    p = self.next_ps()
                    S.op("pe", lambda e: e.matmul(p[:, :], sel[:, e_ * 128:(e_ + 1) * 128],
                                                  self.gatesT[:, g0 + tg * 512:g0 + (tg + 1) * 512], start=True, stop=True),
                         reads=[sel, self.gatesT], writes=[p])
                    S.op("act", lambda e: e.activation(gb[:, tg * 512:(tg + 1) * 512], p[:, :], ACT.Identity),
                         reads=[p], writes=[gb])
                for j in range(KC if dbg.get("gu", True) else 0):
                    w = wgu[wi % 3]
                    wi += 1
                    ws = wst[si % 2]
                    si += 1
                    S.dma("sp", ws[:], self.w_gu.t[li, e_, :, j * 256:(j + 1) * 256].rearrange("(k p) n -> p k n", p=128),
                          reads=[self.w_gu], writes=[ws])
                    S.op("pool", lambda e: e.tensor_copy(w[:], ws[:]), reads=[ws], writes=[w])
                    for tg in range(ntg):
                        pg = self.next_ps()
                        pl = self.next_ps()
                        for half, pp in ((0, pg), (1, pl)):
                            for k in range(KC):
                                S.op("pe", lambda e: e.matmul(pp[:, :], w[:, k, half * 128:(half + 1) * 128],
                                                              fx[:, k, tg * 512:(tg + 1) * 512],
                                                              start=(k == 0), stop=(k == KC - 1)),
                                     reads=[w, fx], writes=[pp])
                        a = t_g[ev % 2]
                        s_ = t_s[ev % 2]
                        l_ = t_l[ev % 2]
                        ev += 1
                        S.op("dve", lambda e: e.tensor_scalar(a[:], pg[:, :], bgu[:, e_, 2 * j:2 * j + 1], 7.0, ALU.add, ALU.min),
                             reads=[pg, bgu], writes=[a])
                        S.op("act", lambda e: e.activation(s_[:], a[:], ACT.Silu, scale=1.702), reads=[a], writes=[s_])
                        S.op("dve", lambda e: e.tensor_scalar(l_[:], pl[:, :], bgu[:, e_, 2 * j + 1:2 * j + 2], None, ALU.add),
                             reads=[pl, bgu], writes=[l_])
                        S.op("pool", lambda e: e.tensor_scalar(l_[:], l_[:], 7.0, -7.0, ALU.min, ALU.max), reads=[l_], writes=[l_])
                        S.op("dve", lambda e: e.scalar_tensor_tensor(s_[:], l_[:], 1.0, s_[:], ALU.add, ALU.mult),
                             reads=[s_, l_], writes=[s_])
                        S.op("dve", lambda e: e.scalar_tensor_tensor(hid[:, j, tg * 512:(tg + 1) * 512], s_[:], 1.0 / 1.702,
                                                                     gb[:, tg * 512:(tg + 1) * 512], ALU.mult, ALU.mult),
                             reads=[s_, gb], writes=[hid])
                for jo in range(4 if dbg.get("dn", True) else 0):
                    w = wdn[di % 3]
                    di += 1
                    ws = wst[si % 2]
                    si += 1
                    S.dma("sp", ws[:], self.w_dn.t[li, e_, :, jo * 256:(jo + 1) * 256].rearrange("(k p) n -> p k n", p=128),
                          reads=[self.w_dn], writes=[ws])
                    S.op("pool", lambda e: e.tensor_copy(w[:], ws[:]), reads=[ws], writes=[w])
                    for m2 in range(2):
                        oc = jo * 2 + m2
                        for tg in range(ntg):
                            p = self.next_ps()
                            for k in range(KC):
                                S.op("pe", lambda e: e.matmul(p[:, :], w[:, k, m2 * 128:(m2 + 1) * 128],
                                                              hid[:, k, tg * 512:(tg + 1) * 512],
                                                              start=(k == 0), stop=(k == KC - 1)),
                                     reads=[w, hid], writes=[p])
                            dst = acc[:, oc, tg * 512:(tg + 1) * 512]
                            if e_ == 0:
                                S.op("act", lambda e: e.activation(dst, p[:, :], ACT.Identity), reads=[p], writes=[acc])
                            else:
                                S.op("dve", lambda e: e.tensor_tensor(dst, p[:, :], dst, ALU.add), reads=[p, acc], writes=[acc])
            for oc in range(KC if dbg.get("bias", True) else 0):
                for tg in range(ntg):
                    p = self.next_ps()
                    S.op("pe", lambda e: e.matmul(p[:, :], bdn[:, oc * 128:(oc + 1) * 128],
                                                  self.gatesT[:, g0 + tg * 512:g0 + (tg + 1) * 512], start=True, stop=True),
                         reads=[bdn, self.gatesT], writes=[p])
                    dst = acc[:, oc, tg * 512:(tg + 1) * 512]
                    S.op("dve", lambda e: e.tensor_tensor(dst, p[:, :], dst, ALU.add), reads=[p, acc], writes=[acc])
            for bi in range(ng // 128):
                t0 = g0 + bi * 128
                b = t0 // NTB
                col = 2 if (t0 - b * NTB) < CTX else b
                h = hb[bi % 2]
                S.dma("sp", h[:], src.t[:, :, t0:t0 + 128], reads=[src], writes=[h])
                for c in range(KC):
                    S.op("dve", lambda e: e.scalar_tensor_tensor(h[:, c, :], acc[:, c, bi * 128:(bi + 1) * 128],
                                                                 self.mod[:, 40 + c, col:col + 1], h[:, c, :], ALU.mult, ALU.add),
                         reads=[acc, self.mod, h], writes=[h])
                S.dma("sp", self.Hs.t[:, :, t0:t0 + 128], h[:], reads=[h], writes=[self.Hs])
        S.barrier()
        st.close()

    def final_norm(self, src):
        S = self.S
        st = ExitStack()
        hb = [S.sbuf(f"fnh{i}", [128, KC, 512], F32, st) for i in range(2)]
        sq = S.sbuf("fn_sq", [128, KC, 512], BF16, st)
        rs = S.sbuf("fn_rs", [128, 512], F32, st)
        fg = S.sbuf("fn_g", [128, KC], F32, st)
        eps_t = S.sbuf("fn_eps", [128, 1], F32, st)
        S.op("pool", lambda e: e.memset(eps_t[:], EPS), writes=[eps_t])
        S.dma("sp", fg[:], self.final_g.t[:, :], reads=[self.final_g], writes=[fg])
        for ti in range(NT // 512):
            t0 = ti * 512
            h = hb[ti % 2]
            S.dma("sp", h[:], src.t[:, :, t0:t0 + 512], reads=[src], writes=[h])
            S.op("act", lambda e: e.activation(sq[:], h[:], ACT.Square), reads=[h], writes=[sq])
            p = self.next_ps()
            for k in range(KC):
                S.op("pe", lambda e: e.matmul(p[:, :], self.ones_bf[:], sq[:, k, :], start=(k == 0), stop=(k == KC - 1)),
                     reads=[self.ones_bf, sq], writes=[p])
            S.op("act", lambda e: e.activation(rs[:], p[:, :], ACT.Sqrt, bias=eps_t[:, 0:1], scale=1.0 / D),
                 reads=[p, eps_t], writes=[rs])
            S.op("dve", lambda e: e.reciprocal(rs[:], rs[:]), reads=[rs], writes=[rs])
            for c in range(KC):
                S.op("dve", lambda e: e.scalar_tensor_tensor(h[:, c, :], h[:, c, :], fg[:, c:c + 1], rs[:], ALU.mult, ALU.mult),
                     reads=[h, fg, rs], writes=[h])
            S.dma("sp", self.OUT.t[:, :, t0:t0 + 512], h[:], reads=[h], writes=[self.OUT])
        S.barrier()
        st.close()


def fm(a):
    n = a.shape[0]
    return np.ascontiguousarray(a.reshape(n, KC, 128).transpose(2, 1, 0))


def prep_shared(inp, layers, ne=NE):
    L = list(layers)
    o = {}
    o["ada_w"] = np.ascontiguousarray(inp["ada_w"][L])
    ab = inp["ada_b"][L].reshape(len(L), 48, 128).transpose(0, 2, 1)
    o["ada_b4"] = np.ascontiguousarray(np.repeat(ab[..., None], 4, axis=-1))
    ng = np.stack([inp["norm_mix_g"][L], inp["norm_ffn_g"][L]], axis=1)
    o["ng"] = np.ascontiguousarray(ng.reshape(len(L), 2, KC, 128).transpose(0, 1, 3, 2))
    o["router_w"] = np.ascontiguousarray(inp["router_w"][L].reshape(len(L), KC, 128, NE).transpose(0, 2, 1, 3))
    o["router_b"] = np.ascontiguousarray(np.broadcast_to(inp["router_b"][L][:, None, :], (len(L), 128, NE)))
    wg = inp["exp_w_gu"][L]
    o["w_gu"] = np.ascontiguousarray(wg.reshape(len(L), ne, D, KC, 128, 2).transpose(0, 1, 2, 3, 5, 4)).reshape(len(L), ne, D, 2 * D)
    bg = inp["exp_b_gu"][L].reshape(len(L), ne, KC, 128, 2).transpose(0, 3, 1, 2, 4)
    o["b_gu"] = np.ascontiguousarray(bg).reshape(len(L), 128, ne, 16)
    o["w_dn"] = np.ascontiguousarray(inp["exp_w_dn"][L])
    o["b_dn"] = np.ascontiguousarray(inp["exp_b_dn"][L])
    o["final_g"] = np.ascontiguousarray(inp["final_g"].reshape(KC, 128).T)
    sel = np.zeros((NE, NE, 128), np.float32)
    for e in range(NE):
        sel[e, e, :] = 1.0
    o["sel"] = sel.reshape(NE, NE * 128)
    o["ident"] = np.eye(128, dtype=np.float32)
    return o


def prep_even(inp, js):
    js = list(js)
    o = {}
    if not js:
        js = [0]
    n = len(js)
    W = inp["ev_w_in"][js]
    pool, q, k, v = W[:, :, 0:256], W[:, :, 256:1024], W[:, :, 1024:1216], W[:, :, 1216:1408]
    perm = np.arange(64).reshape(2, 2, 16)[:, ::-1, :].reshape(64)
    qsw = q.reshape(n, D, 12, 64)[..., perm].reshape(n, D, 768)
    kh = k.reshape(n, D, 3, 64)
    ksw = kh[..., perm]
    k2 = np.concatenate([kh, kh], -1).reshape(n, D, 384)
    ksw2 = np.concatenate([ksw, ksw], -1).reshape(n, D, 384)
    o["ev_w"] = np.ascontiguousarray(np.concatenate([pool, q, qsw, k2, ksw2, v], -1))
    o["ev_wout"] = np.ascontiguousarray(inp["ev_w_out"][js])
    pw = np.zeros((n, 2, 128, 128), np.float32)
    for c in range(2):
        for g in range(2):
            pw[:, c, g * 64:(g + 1) * 64, g * 64:(g + 1) * 64] = inp["pool_w"][js][:, 2 * c + g]
    o["pw_blk"] = pw
    o["pscale"] = np.ascontiguousarray(inp["pool_scale"][js].reshape(n, 2, 128).transpose(0, 2, 1))
    o["sinkcol"] = np.ascontiguousarray(np.broadcast_to(inp["att_sink"][js][:, None, :], (n, 128, 12)))
    return o


def prep_odd(inp, js):
    js = list(js)
    if not js:
        js = [0]
    n = len(js)
    o = {}
    o["od_w"] = np.ascontiguousarray(inp["od_w_in"][js])
    o["od_wout"] = np.ascontiguousarray(inp["od_w_out"][js])
    mu = inp["rw_mu"][js]
    mup = np.zeros((n, 2, 22 * 128), np.float32)
    mup[:, :, 0:2720] = mu
    o["mu_l"] = np.ascontiguousarray(mup.reshape(n, 2, 22, 128).transpose(0, 3, 2, 1))
    o["rw_w2"] = np.ascontiguousarray(inp["rw_w2"][js].reshape(n, 128, 768))
    o["rw_a2"] = np.ascontiguousarray(inp["rw_a2"][js].reshape(n, 128, 768))
    o["rw_g2"] = np.ascontiguousarray(inp["rw_g2"][js])
    vecs = [inp["rw_w0"][js][:, 0], inp["rw_w0"][js][:, 1], inp["rw_a0"][js][:, 0], inp["rw_a0"][js][:, 1],
            inp["rw_k_k"][js], inp["rw_k_a"][js], inp["rw_r_k"][js].reshape(n, 768), inp["rw_gn_g"][js], inp["rw_gn_b"][js]]
    pv = np.stack(vecs, axis=-1)
    o["rw_pv"] = np.ascontiguousarray(pv.reshape(n, 6, 128, 9).transpose(0, 2, 1, 3))
    return o


def const_tables():
    import ml_dtypes
    o = {}
    blk = np.zeros((128, 128), np.float32)
    blk[0:64, 0:64] = 1.0
    blk[64:128, 64:128] = 1.0
    o["blk1"] = blk
    rr = np.arange(128)[:, None]
    cc = np.arange(128)[None, :]
    rm = np.stack([(cc > rr), (cc >= rr), (cc < rr), (cc <= rr)]).astype(np.float32) * blk[None]
    o["rmask"] = np.ascontiguousarray(rm)
    c64 = np.arange(64)
    ph = 2.0 * np.pi * ((c64[:, None] * c64[None, :]) % 64) / 64.0
    d64 = np.zeros((2, 128, 128), np.float64)
    for g in range(2):
        d64[0, g * 64:(g + 1) * 64, g * 64:(g + 1) * 64] = np.cos(ph)
        d64[1, g * 64:(g + 1) * 64, g * 64:(g + 1) * 64] = np.sin(ph)
    o["dft64"] = d64.astype(ml_dtypes.bfloat16)
    for nm, T in (("dftx", SEQ), ("dftc", CTX)):
        t = np.arange(T, dtype=np.int64)
        th = 2.0 * np.pi * ((t[:, None] * t[None, :]) % T) / T
        sc = 1.0 / np.sqrt(64.0 * T)
        tab = np.stack([np.cos(th) * sc, -np.sin(th) * sc])
        tab = tab.reshape(2, T // 128, 128, T).transpose(0, 2, 1, 3)
        o[nm] = np.ascontiguousarray(tab).astype(ml_dtypes.bfloat16)
    t = np.arange(SEQ)
    row = (t // 64).astype(np.float32)
    colp = (t % 64).astype(np.float32)
    inv_freq = (np.float32(10000.0) ** (-np.arange(16, dtype=np.float32) / np.float32(16))).astype(np.float32)
    C = np.zeros((128, SEQ), np.float32)
    Sg = np.zeros((128, SEQ), np.float32)
    for p in range(128):
        dd = p % 64
        a, s_, f = dd // 32, (dd % 32) // 16, dd % 16
        ang = ((row if a == 0 else colp) * inv_freq[f]).astype(np.float32)
        C[p] = np.cos(ang)
        Sg[p] = (-1.0 if s_ == 0 else 1.0) * np.sin(ang)
    o["ropeC"], o["ropeS"] = C, Sg
    qi = np.arange(128)[:, None]
    ki = np.arange(128)[None, :]
    NEG = np.float32(-30000.0)
    mlow = np.where(ki >= qi, 0.0, NEG).astype(np.float32)
    mup = np.where(ki <= qi, 0.0, NEG).astype(np.float32)
    am = np.zeros((3, 128, 640), np.float32)
    am[0, :, 384:512] = mup
    am[1, :, 256:384] = mlow
    am[1, :, 512:640] = mup
    am[2, :, 256:384] = mlow
    o["amask"] = am
    for nm, T in (("inv_x", SEQ), ("inv_c", CTX)):
        inv = np.zeros((128, 2, T), np.float32)
        tt = np.arange(T)
        for c in range(2):
            for g in range(2):
                w = (2, 4, 8, 16)[2 * c + g]
                lo = np.clip(tt - w // 2, 0, T)
                hi = np.clip(tt - w // 2 + w, 0, T)
                inv[g * 64:(g + 1) * 64, c, :] = (1.0 / (hi - lo).astype(np.float32))[None, :]
        o[nm] = inv
    return o


def prep_core(inp, core, h_x=None, h_c=None):
    o = {}
    bs = slice(core * NB, (core + 1) * NB)
    x = inp["x"][bs] if h_x is None else h_x
    c = inp["ctx"][bs] if h_c is None else h_c
    tok = np.concatenate([c, x], axis=1).reshape(NT, D)
    o["h0"] = fm(tok)
    cc = np.zeros((4, D), np.float32)
    cc[0:NB] = inp["c"][bs]
    cc[2] = inp["c_ctx"]
    o["cT"] = np.ascontiguousarray(cc.reshape(4, KC, 128).transpose(2, 1, 0))
    return o


def unfm(a):
    t = a.transpose(2, 1, 0).reshape(NB, NTB, D)
    return t[:, CTX:], t[:, :CTX]


_PROG = {}


def kernel(**inputs):
    inputs = {k: np.asarray(v) for k, v in inputs.items()}
    if "full" not in _PROG:
        _PROG["full"] = Prog()
    prog = _PROG["full"]
    shared = prep_shared(inputs, range(DEPTH))
    shared.update(prep_even(inputs, [0, 1]))
    shared.update(prep_odd(inputs, [0, 1]))
    shared.update(const_tables())
    in_maps = []
    for c in range(NCORES):
        m = dict(shared)
        m.update(prep_core(inputs, c))
        in_maps.append(m)
    res = run_bass_kernel_spmd(prog.nc, in_maps, core_ids=list(range(NCORES)))
    outs = [unfm(np.asarray(r["out"]))[0] for r in res.results]
    return np.ascontiguousarray(np.concatenate(outs, axis=0).astype(np.float32))
```

```python
import numpy as np
from contextlib import ExitStack
import concourse.bass as bass
import concourse.mybir as mybir
from concourse.bass_utils import run_bass_kernel_spmd

F32 = mybir.dt.float32
BF16 = mybir.dt.bfloat16
I32 = mybir.dt.int32
ALU = mybir.AluOpType
ACT = mybir.ActivationFunctionType
AX = mybir.AxisListType

SAME_ENGINE_SYNC = True


class Res:
    __slots__ = ("name", "t", "lw", "rd")

    def __init__(self, name, t=None):
        self.name = name
        self.t = t
        self.lw = None
        self.rd = {}

    def __getitem__(self, idx):
        return self.t[idx]


class Sched:
    NDMA = {"sp": 40, "pool": 16, "act": 8}

    def __init__(self, nc):
        self.nc = nc
        self.stack = ExitStack()
        self.E = {"pe": nc.tensor, "dve": nc.vector, "act": nc.scalar, "pool": nc.gpsimd, "sp": nc.sync}
        self.sem = {}
        self.val = {}
        for k in ("pe", "dve", "act", "pool"):
            self.sem[k] = self.stack.enter_context(nc.semaphore("s_" + k))
            self.val[k] = 0
        self.rr = {}
        for q, n in self.NDMA.items():
            self.rr[q] = 0
            for i in range(n):
                self.sem[(q, i)] = self.stack.enter_context(nc.semaphore(f"d_{q}{i}"))
                self.val[(q, i)] = 0
        self.known = {e: {} for e in self.E}
        self.n_inst = 0
        self.n_wait = 0

    def sbuf(self, name, shape, dtype, st=None):
        self.uid = getattr(self, "uid", 0) + 1
        t = (st or self.stack).enter_context(self.nc.sbuf_tensor(f"{name}_{self.uid}", list(shape), dtype))
        return Res(name, t)

    def barrier(self):
        for eng, e in self.E.items():
            kn = self.known[eng]
            for k, v in self.val.items():
                if v > 0 and kn.get(k, 0) < v and k != eng:
                    e.wait_ge(self.sem[k], v)
                    kn[k] = v
                    self.n_wait += 1

    def psum(self, name, shape, dtype=F32):
        t = self.stack.enter_context(self.nc.psum_tensor(name, list(shape), dtype))
        return Res(name, t)

    def dram(self, name, shape, dtype, kind="Internal"):
        t = self.nc.dram_tensor(name, list(shape), dtype, kind=kind)
        return Res(name, t.ap())

    def view(self, name):
        return Res(name, None)

    def _need(self, eng, reads, writes):
        need = {}

        def add(ev):
            if ev is None:
                return
            k, v = ev
            if need.get(k, 0) < v:
                need[k] = v
        for r in reads:
            add(r.lw)
        for w in writes:
            add(w.lw)
            for k, v in w.rd.items():
                add((k, v))
        e = self.E[eng]
        kn = self.known[eng]
        for k, v in need.items():
            if k == eng:
                if not SAME_ENGINE_SYNC or eng == "pe":
                    continue
            if kn.get(k, 0) < v:
                e.wait_ge(self.sem[k], v)
                kn[k] = v
                self.n_wait += 1

    def _mark(self, ev, reads, writes):
        k, v = ev
        for r in reads:
            if r.rd.get(k, 0) < v:
                r.rd[k] = v
        for w in writes:
            w.lw = ev
            w.rd = {}

    def op(self, eng, fn, reads=(), writes=()):
        self._need(eng, reads, writes)
        inst = fn(self.E[eng])
        self.val[eng] += 1
        inst.then_inc(self.sem[eng], 1)
        self._mark((eng, self.val[eng]), reads, writes)
        self.n_inst += 1
        return inst

    def dma(self, q, out, in_, reads=(), writes=(), **kw):
        e = self.E[q]
        i = self.rr[q]
        self.rr[q] = (i + 1) % self.NDMA[q]
        k = (q, i)
        kn = self.known[q]
        if kn.get(k, 0) < self.val[k]:
            e.wait_ge(self.sem[k], self.val[k])
            kn[k] = self.val[k]
            self.n_wait += 1
        self._need(q, reads, writes)
        inst = e.dma_start(out=out, in_=in_, **kw)
        self.val[k] += 16
        inst.then_inc(self.sem[k], 16)
        self._mark((k, self.val[k]), reads, writes)
        self.n_inst += 1
        return inst

    def finish(self):
        e = self.E["sp"]
        kn = self.known["sp"]
        for k, v in self.val.items():
            if v > 0 and kn.get(k, 0) < v:
                e.wait_ge(self.sem[k], v)
                kn[k] = v
        self.stack.close()


D = 1024
KC = 8
NB = 2
CTX = 256
SEQ = 2048
NTB = CTX + SEQ
NT = NB * NTB
DEPTH = 4
NE = 32
EPS = 1e-5
NCORES = 8


def token_tiles():
    tl = []
    for b in range(NB):
        tl.append((b * NTB, CTX, 2, b, True))
        for i in range(SEQ // 512):
            tl.append((b * NTB + CTX + 512 * i, 512, b, b, False))
    return tl


class Prog:
    def __init__(self, layers=range(DEPTH), do_mixer=True, do_ffn=True, do_final=True, dbg=None):
        self.dbg = dbg or {}
        self.layers = list(layers)
        self.do_mixer = do_mixer
        self.do_ffn = do_ffn
        self.do_final = do_final
        self.nc = bass.Bass("TRN2", target_bir_lowering=False)
        self.S = Sched(self.nc)
        self.inputs = {}
        self._build()

    def inp(self, name, shape, dtype=F32):
        r = self.S.dram(name, shape, dtype, kind="ExternalInput")
        self.inputs[name] = r
        return r

    def _build(self):
        S = self.S
        self.H = self.inp("h0", [128, KC, NT])
        self.Hs = S.dram("Hs", [128, KC, NT], F32)
        self.OUT = S.dram("out", [128, KC, NT], F32, kind="ExternalOutput")
        self.FX = S.dram("FX", [128, KC, NT], BF16)
        self.cT = self.inp("cT", [128, KC, 4])
        NL = len(self.layers)
        self.lidx = {li: i for i, li in enumerate(self.layers)}
        self.ada_w = self.inp("ada_w", [NL, D, 6 * D])
        self.ada_b4 = self.inp("ada_b4", [NL, 128, 48, 4])
        self.ng = self.inp("ng", [NL, 2, 128, KC])
        self.router_w = self.inp("router_w", [NL, 128, KC, NE])
        self.router_b = self.inp("router_b", [NL, 128, NE])
        NEX = 1 if self.dbg.get("nomoe_w") else NE
        self.w_gu = self.inp("w_gu", [NL, NEX, D, 2 * D])
        self.b_gu = self.inp("b_gu", [NL, 128, NEX, 16])
        self.w_dn = self.inp("w_dn", [NL, NEX, D, D])
        self.b_dn = self.inp("b_dn", [NL, NEX, D])
        self.final_g = self.inp("final_g", [128, KC])
        NEV = max(1, len([l for l in self.layers if l % 2 == 0]))
        self.eidx = {l: i for i, l in enumerate([l for l in self.layers if l % 2 == 0])}
        self.ev_w = self.inp("ev_w", [NEV, D, 2752])
        self.ev_wout = self.inp("ev_wout", [NEV, D, D])
        self.pw_blk = self.inp("pw_blk", [NEV, 2, 128, 128])
        self.pscale = self.inp("pscale", [NEV, 128, 2])
        self.sinkcol = self.inp("sinkcol", [NEV, 128, 12])
        self.ropeC = self.inp("ropeC", [128, SEQ])
        self.ropeS = self.inp("ropeS", [128, SEQ])
        self.amask = self.inp("amask", [3, 128, 640])
        self.inv_x = self.inp("inv_x", [128, 2, SEQ])
        self.inv_c = self.inp("inv_c", [128, 2, CTX])
        NOD = max(1, len([l for l in self.layers if l % 2 == 1]))
        self.oidx = {l: i for i, l in enumerate([l for l in self.layers if l % 2 == 1])}
        self.od_w = self.inp("od_w", [NOD, D, 2976])
        self.od_wout = self.inp("od_wout", [NOD, D, D])
        self.mu_l = self.inp("mu_l", [NOD, 128, 22, 2])
        self.rw_w2 = self.inp("rw_w2", [NOD, 128, 768])
        self.rw_a2 = self.inp("rw_a2", [NOD, 128, 768])
        self.rw_g2 = self.inp("rw_g2", [NOD, 160, 768])
        self.rw_pv = self.inp("rw_pv", [NOD, 128, 6, 9])
        self.blk1 = self.inp("blk1", [128, 128])
        self.rmask = self.inp("rmask", [4, 128, 128])
        self.dft64 = self.inp("dft64", [2, 128, 128], BF16)
        self.dftx = self.inp("dftx", [2, 128, SEQ // 128, SEQ], BF16)
        self.dftc = self.inp("dftc", [2, 128, CTX // 128, CTX], BF16)
        self.F = S.dram("Fs", [128, 22, NT], BF16)
        self.FN = S.dram("FNs", [128, 2, NT], BF16)
        self.Q = S.dram("Qs", [128, 6, NT], BF16)
        self.K2 = S.dram("K2s", [128, 3, NT], BF16)
        self.V = S.dram("Vs", [128, NT // 128, 192], BF16)
        self.U = S.dram("Us", [128, 2, NT], F32)
        self.MIX = S.dram("MIXs", [128, KC, NT], BF16)
        self.sel = self.inp("sel", [NE, NE * 128])
        self.ident_in = self.inp("ident", [128, 128])

        self.ps2 = [S.psum(f"psd{i}", [128, 1024], F32) for i in range(4)]
        self.ps = []
        for i in range(4):
            for hh in range(2):
                self.ps.append(Res(f"ps{2 * i + hh}", self.ps2[i].t[:, hh * 512:(hh + 1) * 512]))
        self.ps_set = list(range(8))
        self.ps_rr = 0
        self.ones_bf = S.sbuf("ones_bf", [128, 128], BF16)
        S.op("pool", lambda e: e.memset(self.ones_bf[:], 1.0), writes=[self.ones_bf])
        self.ident = S.sbuf("ident", [128, 128], F32)
        S.dma("sp", self.ident[:], self.ident_in.t[:, :], reads=[self.ident_in], writes=[self.ident])
        self.silu_c = S.sbuf("silu_c", [128, KC, 4], F32)
        S.dma("sp", self.silu_c[:], self.cT.t[:, :, :], reads=[self.cT], writes=[self.silu_c])
        S.op("act", lambda e: e.activation(self.silu_c[:], self.silu_c[:], ACT.Silu),
             reads=[self.silu_c], writes=[self.silu_c])
        self.mod = S.sbuf("mod", [128, 48, 4], F32)
        self.gs = S.sbuf("gs", [128, 2, KC, 4], F32)
        self.ngs = S.sbuf("ngs", [128, 2, KC], F32)
        self.gneps = S.sbuf("gneps", [128, 1], F32)
        S.op("pool", lambda e: e.memset(self.gneps[:], 64e-5), writes=[self.gneps])

        first = True
        for li_ in self.layers:
            li = self.lidx[li_]
            self.adaln(li)
            src = self.H if first else self.Hs
            if self.do_mixer:
                if li_ % 2 == 0:
                    self.even_mixer(self.eidx[li_], src)
                else:
                    self.odd_mixer(self.oidx[li_], src)
                first = False
                src = self.Hs
            if self.do_ffn:
                st_ffn = ExitStack()
                self.gatesT = S.sbuf("gatesT", [NE, NT], F32, st_ffn)
                last = (li_ == DEPTH - 1)
                grp = None
                if self.dbg.get("norm", True):
                    self.norm_mod(li, 1, src, router=True, skip_ctx=False)
                if self.dbg.get("moe", True):
                    self.moe(li, src, grp)
                    first = False
                S.barrier()
                st_ffn.close()
        if self.do_final:
            self.final_norm(self.H if first else self.Hs)
        else:
            src = self.H if first else self.Hs
            for c in range(KC):
                S.dma("sp", self.OUT.t[:, c, :], src.t[:, c, :], reads=[src], writes=[self.OUT])
        S.finish()

    def next_ps(self):
        self.ps_rr = (self.ps_rr + 1) % len(self.ps_set)
        return self.ps[self.ps_set[self.ps_rr]]

    def adaln(self, li):
        S = self.S
        with ExitStack() as st:
            wbuf = [S.sbuf(f"adaw{i}", [128, KC, 512], F32, st) for i in range(2)]
            adab = S.sbuf("adab", [128, 48, 4], F32, st)
            S.dma("sp", adab[:], self.ada_b4.t[li], reads=[self.ada_b4], writes=[adab])
            S.dma("sp", self.ngs[:], self.ng.t[li].rearrange("a p c -> p a c"), reads=[self.ng], writes=[self.ngs])
            p = self.next_ps()
            for blk in range(12):
                w = wbuf[blk % 2]
                S.dma("sp", w[:], self.ada_w.t[li, :, blk * 512:(blk + 1) * 512].rearrange("(k p) n -> p k n", p=128),
                      reads=[self.ada_w], writes=[w])
                for mm in range(4):
                    m = blk * 4 + mm
                    for k in range(KC):
                        S.op("pe", lambda e: e.matmul(p[:, m * 4:(m + 1) * 4], w[:, k, mm * 128:(mm + 1) * 128],
                                                      self.silu_c[:, k, :], start=(k == 0), stop=(k == KC - 1)),
                             reads=[w, self.silu_c], writes=[p])
            S.op("dve", lambda e: e.tensor_tensor(self.mod[:].rearrange("p a b -> p (a b)"), p[:, 0:192],
                                                  adab[:].rearrange("p a b -> p (a b)"), ALU.add),
                 reads=[p, adab], writes=[self.mod])
            for which, base in ((0, 8), (1, 32)):
                for c in range(KC):
                    S.op("dve", lambda e: e.tensor_scalar(self.gs[:, which, c, :], self.mod[:, base + c, :], 1.0,
                                                          self.ngs[:, which, c:c + 1], ALU.add, ALU.mult),
                         reads=[self.mod, self.ngs], writes=[self.gs])
            S.barrier()

    def norm_mod_tile(self, which, h, t0, n, col, out_bf, out_f32=None, ooff=0):
        S = self.S
        sq = self.nm_sq
        S.op("act", lambda e: e.activation(sq[:, :, 0:n], h[:, :, 0:n], ACT.Square), reads=[h], writes=[sq])
        p = self.next_ps()
        for k in range(KC):
            S.op("pe", lambda e: e.matmul(p[:, 0:n], self.ones_bf[:], sq[:, k, 0:n], start=(k == 0), stop=(k == KC - 1)),
                 reads=[self.ones_bf, sq], writes=[p])
        rs = self.nm_rs
        S.op("act", lambda e: e.activation(rs[:, 0:n], p[:, 0:n], ACT.Sqrt, bias=self.eps_t[:, 0:1], scale=1.0 / D),
             reads=[p, self.eps_t], writes=[rs])
        S.op("dve", lambda e: e.reciprocal(rs[:, 0:n], rs[:, 0:n]), reads=[rs], writes=[rs])
        shb = 0 if which == 0 else 24
        for c in range(KC):
            tmp = self.nm_tmp[c % 2]
            S.op("dve", lambda e: e.scalar_tensor_tensor(tmp[:, 0:n], h[:, c, 0:n], self.gs[:, which, c, col:col + 1],
                                                         rs[:, 0:n], ALU.mult, ALU.mult),
                 reads=[h, self.gs, rs], writes=[tmp])
            S.op("act", lambda e: e.activation(out_bf[:, c, ooff:ooff + n], tmp[:, 0:n], ACT.Identity,
                                               bias=self.mod[:, shb + c, col:col + 1]),
                 reads=[tmp, self.mod], writes=[out_bf])
            if out_f32 is not None:
                S.op("pool", lambda e: e.tensor_scalar(out_f32[:, c, 0:n], tmp[:, 0:n], self.mod[:, shb + c, col:col + 1],
                                                       None, ALU.add),
                     reads=[tmp, self.mod], writes=[out_f32])

    def nm_alloc(self, st):
        S = self.S
        self.nm_sq = S.sbuf("nm_sq", [128, KC, 512], BF16, st)
        self.nm_rs = S.sbuf("nm_rs", [128, 512], F32, st)
        self.nm_tmp = [S.sbuf(f"nm_tmp{i}", [128, 512], F32, st) for i in range(2)]
        self.eps_t = S.sbuf("eps_t", [128, 1], F32, st)
        S.op("pool", lambda e: e.memset(self.eps_t[:], EPS), writes=[self.eps_t])

    def norm_mod(self, li, which, src, router, skip_ctx=False):
        S = self.S
        st = ExitStack()
        self.nm_alloc(st)
        hb = [S.sbuf(f"nmh{i}", [128, KC, 512], F32, st) for i in range(2)]
        ob = [S.sbuf(f"nmo{i}", [128, KC, 512], BF16, st) for i in range(2)]
        of = [S.sbuf(f"nmf{i}", [128, KC, 512], F32, st) for i in range(2)]
        rw = S.sbuf("rw", [128, KC, NE], F32, st)
        rb = S.sbuf("rb", [128, NE], F32, st)
        S.dma("sp", rw[:], self.router_w.t[li], reads=[self.router_w], writes=[rw])
        S.dma("sp", rb[:], self.router_b.t[li], reads=[self.router_b], writes=[rb])
        rbuf = [[S.sbuf(f"rt{i}_{nm}", shp, F32, st) for nm, shp in
                 (("lg", [128, NE]), ("m8", [128, 8]), ("nmx", [128, 1]), ("msk", [128, NE]), ("ex", [128, NE]), ("ssum", [128, 1]))]
                for i in range(4)]
        for ti, (t0, n, col, b, isc) in enumerate(token_tiles()):
            if isc and skip_ctx:
                continue
            h = hb[ti % 2]
            o = ob[ti % 2]
            f = of[ti % 2]
            S.dma("sp", h[:, :, 0:n], src.t[:, :, t0:t0 + n], reads=[src], writes=[h])
            self.norm_mod_tile(which, h, t0, n, col, o, f)
            S.dma("sp", self.FX.t[:, :, t0:t0 + n], o[:, :, 0:n], reads=[o], writes=[self.FX])
            def router_gen(s_):
                lg_, m8_, nmx_, msk_, ex_, ssum_ = rbuf[s_]
                p = self.next_ps()
                for k in range(KC):
                    S.op("pe", lambda e: e.matmul(p[:, 0:NE], f[:, k, s_ * 128:(s_ + 1) * 128], rw[:, k, :],
                                                  start=(k == 0), stop=(k == KC - 1)),
                         reads=[f, rw], writes=[p])
                yield
                S.op("dve", lambda e: e.tensor_tensor(lg_[:], p[:, 0:NE], rb[:], ALU.add), reads=[p, rb], writes=[lg_])
                S.op("dve", lambda e: e.max(m8_[:], lg_[:]), reads=[lg_], writes=[m8_])
                S.op("dve", lambda e: e.tensor_scalar(msk_[:], lg_[:], m8_[:, 3:4], None, ALU.is_ge), reads=[lg_, m8_], writes=[msk_])
                S.op("dve", lambda e: e.tensor_scalar(nmx_[:], m8_[:, 0:1], -1.0, None, ALU.mult), reads=[m8_], writes=[nmx_])
                yield
                S.op("act", lambda e: e.activation(ex_[:], lg_[:], ACT.Exp, bias=nmx_[:, 0:1]), reads=[lg_, nmx_], writes=[ex_])
                yield
                S.op("dve", lambda e: e.tensor_tensor(ex_[:], ex_[:], msk_[:], ALU.mult), reads=[ex_, msk_], writes=[ex_])
                S.op("dve", lambda e: e.reduce_sum(ssum_[:], ex_[:], AX.X), reads=[ex_], writes=[ssum_])
                S.op("dve", lambda e: e.reciprocal(ssum_[:], ssum_[:]), reads=[ssum_], writes=[ssum_])
                S.op("dve", lambda e: e.tensor_scalar(ex_[:], ex_[:], ssum_[:, 0:1], None, ALU.mult), reads=[ex_, ssum_], writes=[ex_])
                yield
                p2 = self.next_ps()
                S.op("pe", lambda e: e.transpose(p2[0:NE, 0:128], ex_[:], self.ident[:]), reads=[ex_, self.ident], writes=[p2])
                yield
                tt = t0 + s_ * 128
                S.op("act", lambda e: e.activation(self.gatesT[:, tt:tt + 128], p2[0:NE, 0:128], ACT.Identity),
                     reads=[p2], writes=[self.gatesT])
            gens = [router_gen(s_) for s_ in range(n // 128)]
            while gens:
                for g_ in list(gens):
                    try:
                        next(g_)
                    except StopIteration:
                        gens.remove(g_)
        S.barrier()
        st.close()

    def even_proj(self, je, src):
        S = self.S
        st = ExitStack()
        self.nm_alloc(st)
        W = S.sbuf("evW", [128, KC, 2752], BF16, st)
        for k in range(KC):
            S.dma("pool", W[:, k, :], self.ev_w.t[je, k * 128:(k + 1) * 128, :], reads=[self.ev_w], writes=[W])
        rC = S.sbuf("ropeC", [128, SEQ], F32, st)
        rS = S.sbuf("ropeS", [128, SEQ], F32, st)
        S.dma("sp", rC[:], self.ropeC.t[:, :], reads=[self.ropeC], writes=[rC])
        S.dma("sp", rS[:], self.ropeS.t[:, :], reads=[self.ropeS], writes=[rS])
        hb = [S.sbuf(f"eph{i}", [128, KC, 512], F32, st) for i in range(2)]
        ob = [S.sbuf(f"epo{i}", [128, KC, 512], BF16, st) for i in range(2)]
        r1 = [S.sbuf(f"epr1{i}", [128, 512], F32, st) for i in range(2)]
        r2 = [S.sbuf(f"epr2{i}", [128, 512], F32, st) for i in range(2)]
        qo = [S.sbuf(f"epq{i}", [128, 512], BF16, st) for i in range(3)]
        uo = [S.sbuf(f"epu{i}", [128, 512], F32, st) for i in range(2)]
        vo = [S.sbuf(f"epv{i}", [128, 192], BF16, st) for i in range(2)]
        qi = 0
        for ti, (t0, n, col, b, isc) in enumerate(token_tiles()):
            h = hb[ti % 2]
            o = ob[ti % 2]
            S.dma("sp", h[:, :, 0:n], src.t[:, :, t0:t0 + n], reads=[src], writes=[h])
            self.norm_mod_tile(0, h, t0, n, col, o)

            def proj(p, off):
                for k in range(KC):
                    S.op("pe", lambda e: e.matmul(p[:, 0:n], W[:, k, off:off + 128], o[:, k, 0:n],
                                                  start=(k == 0), stop=(k == KC - 1)), reads=[W, o], writes=[p])
            for c in range(2):
                p = self.next_ps()
                proj(p, c * 128)
                u = uo[c]
                S.op("act", lambda e: e.activation(u[:, 0:n], p[:, 0:n], ACT.Identity), reads=[p], writes=[u])
                S.dma("sp", self.U.t[:, c, t0:t0 + n], u[:, 0:n], reads=[u], writes=[self.U])
            items = [(self.Q, m, 256 + m * 128, 1024 + m * 128) for m in range(6)] + \
                    [(self.K2, g, 1792 + g * 128, 2176 + g * 128) for g in range(3)]
            for (dst, dch, off, offsw) in items:
                q = qo[qi % 3]
                qi += 1
                p = self.next_ps()
                proj(p, off)
                if isc:
                    S.op("act", lambda e: e.activation(q[:, 0:n], p[:, 0:n], ACT.Identity), reads=[p], writes=[q])
                else:
                    p2 = self.next_ps()
                    proj(p2, offsw)
                    pos0 = t0 - b * NTB - CTX
                    a1 = r1[qi % 2]
                    a2 = r2[qi % 2]
                    S.op("dve", lambda e: e.tensor_tensor(a1[:, 0:n], p[:, 0:n], rC[:, pos0:pos0 + n], ALU.mult),
                         reads=[p, rC], writes=[a1])
                    S.op("dve", lambda e: e.tensor_tensor(a2[:, 0:n], p2[:, 0:n], rS[:, pos0:pos0 + n], ALU.mult),
                         reads=[p2, rS], writes=[a2])
                    S.op("pool", lambda e: e.tensor_tensor(q[:, 0:n], a1[:, 0:n], a2[:, 0:n], ALU.add),
                         reads=[a1, a2], writes=[q])
                S.dma("sp", dst.t[:, dch, t0:t0 + n], q[:, 0:n], reads=[q], writes=[dst])
            for s_ in range(n // 128):
                p = self.next_ps()
                for k in range(KC):
                    S.op("pe", lambda e: e.matmul(p[:, 0:192], o[:, k, s_ * 128:(s_ + 1) * 128], W[:, k, 2560:2752],
                                                  start=(k == 0), stop=(k == KC - 1)), reads=[W, o], writes=[p])
                v = vo[s_ % 2]
                S.op("act", lambda e: e.activation(v[:], p[:, 0:192], ACT.Identity), reads=[p], writes=[v])
                S.dma("sp", self.V.t[:, (t0 + s_ * 128) // 128, :], v[:], reads=[v], writes=[self.V])
        S.barrier()
        st.close()

    def even_attn(self, je):
        S = self.S
        st = ExitStack()
        self.ps_set = [6, 7]
        scp = [Res("scA", self.ps2[0].t), Res("scB", self.ps2[1].t)]
        masks = S.sbuf("amask", [128, 3, 640], F32, st)
        S.dma("sp", masks[:], self.amask.t.rearrange("v p n -> p v n"), reads=[self.amask], writes=[masks])
        sink = S.sbuf("sink", [128, 12], F32, st)
        S.dma("sp", sink[:], self.sinkcol.t[je], reads=[self.sinkcol], writes=[sink])
        Qb = S.sbuf("Qb", [128, 6, NTB], BF16, st)
        Kb = S.sbuf("Kb", [128, 3, NTB], BF16, st)
        Vb = S.sbuf("Vb", [128, NTB // 128, 192], BF16, st)
        att = S.sbuf("attb", [128, 6, NTB], BF16, st)
        ssb = [S.sbuf(f"ssb{i}", [128, 640], F32, st) for i in range(2)]
        eeb = [S.sbuf(f"eeb{i}", [128, 640], F32, st) for i in range(2)]
        pnb = [S.sbuf(f"pnb{i}", [128, 640], F32, st) for i in range(2)]
        ptb = [S.sbuf(f"ptb{i}", [128, 640], BF16, st) for i in range(2)]
        sm = [[S.sbuf(f"sm{i}_{j}", [128, 1], F32, st) for j in range(5)] for i in range(2)]
        tpp = [Res("tppA", self.ps2[2].t), Res("tppB", self.ps2[3].t)]

        def item_gen(h, blk, slot):
            g = h // 4
            hp = (h % 2) * 64
            qc = h // 2
            if blk < 0:
                q0 = (blk + 2) * 128
                segs = [(0, 256, 0)]
                mv = None
            else:
                q0 = CTX + blk * 128
                kb = [x for x in (blk - 1, blk, blk + 1) if 0 <= x < SEQ // 128]
                segs = [(0, 256, 0)] + [(CTX + x * 128, 128, 256 + i * 128) for i, x in enumerate(kb)]
                mv = 0 if blk == 0 else (2 if blk == SEQ // 128 - 1 else 1)
            ncol = sum(x[1] for x in segs)
            sc = scp[slot]
            tp = tpp[slot]
            ss = ssb[slot]
            ee = eeb[slot]
            pn = pnb[slot]
            pt = ptb[slot]
            mx, nmx, rsum, es, den = sm[slot]
            for (k0, kn, c0) in segs:
                S.op("pe", lambda e: e.matmul(sc[:, c0:c0 + kn], Qb[hp:hp + 64, qc, q0:q0 + 128],
                                              Kb[hp:hp + 64, g, k0:k0 + kn], start=True, stop=True),
                     reads=[Qb, Kb], writes=[sc])
            yield
            if mv is None:
                S.op("act", lambda e: e.activation(ss[:, 0:ncol], sc[:, 0:ncol], ACT.Identity), reads=[sc], writes=[ss])
            else:
                S.op("dve", lambda e: e.tensor_tensor(ss[:, 0:ncol], sc[:, 0:ncol], masks[:, mv, 0:ncol], ALU.add),
                     reads=[sc, masks], writes=[ss])
            S.op("dve", lambda e: e.reduce_max(mx[:], ss[:, 0:ncol], AX.X), reads=[ss], writes=[mx])
            S.op("dve", lambda e: e.tensor_scalar(nmx[:], mx[:], -0.125, None, ALU.mult), reads=[mx], writes=[nmx])
            yield
            S.op("act", lambda e: e.activation(ee[:, 0:ncol], ss[:, 0:ncol], ACT.Exp, bias=nmx[:, 0:1], scale=0.125,
                                               accum_out=rsum[:, 0:1]), reads=[ss, nmx], writes=[ee, rsum])
            S.op("act", lambda e: e.activation(es[:], nmx[:], ACT.Exp, bias=sink[:, h:h + 1]),
                 reads=[nmx, sink], writes=[es])
            yield
            S.op("dve", lambda e: e.tensor_tensor(den[:], rsum[:], es[:], ALU.add), reads=[rsum, es], writes=[den])
            S.op("dve", lambda e: e.reciprocal(den[:], den[:]), reads=[den], writes=[den])
            yield
            S.op("act", lambda e: e.activation(pn[:, 0:ncol], ee[:, 0:ncol], ACT.Copy, scale=den[:, 0:1]),
                 reads=[ee, den], writes=[pn])
            yield
            nbk = ncol // 128
            for i in range(nbk):
                S.op("pe", lambda e: e.transpose(tp[:, i * 128:(i + 1) * 128], pn[:, i * 128:(i + 1) * 128], self.ident[:]),
                     reads=[pn, self.ident], writes=[tp])
            yield
            S.op("dve", lambda e: e.tensor_copy(pt[:, 0:ncol], tp[:, 0:ncol]), reads=[tp], writes=[pt])
            yield
            kts = []
            for (k0, kn, c0) in segs:
                for j in range(kn // 128):
                    kts.append(k0 // 128 + j)
            for i, kt in enumerate(kts):
                S.op("pe", lambda e: e.matmul(sc[hp:hp + 64, 0:128], Vb[:, kt, g * 64:(g + 1) * 64],
                                              pt[:, i * 128:(i + 1) * 128], start=(i == 0), stop=(i == nbk - 1)),
                     reads=[Vb, pt], writes=[sc])
            yield
            S.op("act", lambda e: e.activation(att[hp:hp + 64, qc, q0:q0 + 128], sc[hp:hp + 64, 0:128], ACT.Identity),
                 reads=[sc], writes=[att])

        for b in range(NB):
            b0 = b * NTB
            S.dma("sp", Qb[:], self.Q.t[:, :, b0:b0 + NTB], reads=[self.Q], writes=[Qb])
            S.dma("sp", Kb[:], self.K2.t[:, :, b0:b0 + NTB], reads=[self.K2], writes=[Kb])
            S.dma("sp", Vb[:], self.V.t[:, b0 // 128:(b0 + NTB) // 128, :], reads=[self.V], writes=[Vb])
            todo = [(h, blk) for h in range(12) for blk in range(-2, SEQ // 128)]
            active = {}
            ti_ = 0
            while ti_ < len(todo) or active:
                for slot in (0, 1):
                    if slot not in active and ti_ < len(todo):
                        active[slot] = item_gen(todo[ti_][0], todo[ti_][1], slot)
                        ti_ += 1
                        if slot == 1 and len(active) == 2 and ti_ == 2:
                            for _ in range(4):
                                next(active[0])
                for slot in list(active):
                    try:
                        next(active[slot])
                    except StopIteration:
                        del active[slot]
            S.dma("sp", self.MIX.t[:, 2:8, b0:b0 + NTB], att[:], reads=[att], writes=[self.MIX])
        S.barrier()
        self.ps_set = list(range(8))
        st.close()

    def even_pool(self, je):
        S = self.S
        st = ExitStack()
        TM = SEQ
        A = [S.sbuf(f"plA{i}", [128, TM + 16], F32, st) for i in range(5)]
        prod = S.sbuf("plprod", [128, TM], F32, st)
        pooled = S.sbuf("plpooled", [128, TM], BF16, st)
        invx = S.sbuf("plinvx", [128, 2, SEQ], F32, st)
        invc = S.sbuf("plinvc", [128, 2, CTX], F32, st)
        PW = S.sbuf("plW", [128, 2, 128], BF16, st)
        psc = S.sbuf("plsc", [128, 2], F32, st)
        mo = [S.sbuf(f"plmo{i}", [128, 512], BF16, st) for i in range(2)]
        S.dma("sp", invx[:], self.inv_x.t[:, :, :], reads=[self.inv_x], writes=[invx])
        S.dma("sp", invc[:], self.inv_c.t[:, :, :], reads=[self.inv_c], writes=[invc])
        S.dma("pool", PW[:], self.pw_blk.t[je].rearrange("c p n -> p c n"), reads=[self.pw_blk], writes=[PW])
        S.dma("sp", psc[:], self.pscale.t[je], reads=[self.pscale], writes=[psc])
        oi = 0
        for b in range(NB):
            for isc in (True, False):
                T = CTX if isc else SEQ
                t0 = b * NTB + (0 if isc else CTX)
                inv = invc if isc else invx
                for c in range(2):
                    for a in A:
                        S.op("pool", lambda e: e.memset(a[:, 0:8], 0.0), writes=[a])
                        S.op("pool", lambda e: e.memset(a[:, T + 8:T + 16], 0.0), writes=[a])
                    S.dma("sp", A[0][:, 8:T + 8], self.U.t[:, c, t0:t0 + T], reads=[self.U], writes=[A[0]])
                    shifts = [(0, 1), (1, 1), (2, 2), (4, 4)]
                    nlev = 2 if c == 0 else 4
                    for lv in range(nlev):
                        sp_, sm_ = shifts[lv]
                        a_in = A[lv]
                        a_out = A[lv + 1]
                        S.op("dve", lambda e: e.tensor_tensor(a_out[:, 8:T + 8], a_in[:, 8 + sp_:T + 8 + sp_],
                                                              a_in[:, 8 - sm_:T + 8 - sm_], ALU.add),
                             reads=[a_in], writes=[a_out])
                    lo = A[1] if c == 0 else A[3]
                    hi = A[2] if c == 0 else A[4]
                    S.op("dve", lambda e: e.tensor_tensor(prod[0:64, 0:T], lo[0:64, 8:T + 8], inv[0:64, c, 0:T], ALU.mult),
                         reads=[lo, inv], writes=[prod])
                    S.op("dve", lambda e: e.tensor_tensor(prod[64:128, 0:T], hi[64:128, 8:T + 8], inv[64:128, c, 0:T], ALU.mult),
                         reads=[hi, inv], writes=[prod])
                    S.op("pool", lambda e: e.tensor_tensor(pooled[:, 0:T], prod[:, 0:T], A[0][:, 8:T + 8], ALU.subtract),
                         reads=[prod, A[0]], writes=[pooled])
                    for n0 in range(0, T, 512):
                        n = min(512, T - n0)
                        p = self.next_ps()
                        S.op("pe", lambda e: e.matmul(p[:, 0:n], PW[:, c, :], pooled[:, n0:n0 + n], start=True, stop=True),
                             reads=[PW, pooled], writes=[p])
                        m_ = mo[oi % 2]
                        oi += 1
                        S.op("act", lambda e: e.activation(m_[:, 0:n], p[:, 0:n], ACT.Copy, scale=psc[:, c:c + 1]),
                             reads=[p, psc], writes=[m_])
                        S.dma("sp", self.MIX.t[:, c, t0 + n0:t0 + n0 + n], m_[:, 0:n], reads=[m_], writes=[self.MIX])
        S.barrier()
        st.close()

    def mix_out(self, w_ap, wres, src):
        S = self.S
        st = ExitStack()
        Wo = S.sbuf("moW", [128, KC, D], BF16, st)
        for k in range(KC):
            S.dma("pool", Wo[:, k, :], w_ap[k * 128:(k + 1) * 128, :], reads=[wres], writes=[Wo])
        hb = [S.sbuf(f"moh{i}", [128, KC, 512], F32, st) for i in range(2)]
        mb = [S.sbuf(f"mom{i}", [128, KC, 512], BF16, st) for i in range(2)]
        for ti, (t0, n, col, b, isc) in enumerate(token_tiles()):
            h = hb[ti % 2]
            mx = mb[ti % 2]
            S.dma("sp", h[:, :, 0:n], src.t[:, :, t0:t0 + n], reads=[src], writes=[h])
            S.dma("sp", mx[:, :, 0:n], self.MIX.t[:, :, t0:t0 + n], reads=[self.MIX], writes=[mx])
            for m in range(KC):
                p = self.next_ps()
                for k in range(KC):
                    S.op("pe", lambda e: e.matmul(p[:, 0:n], Wo[:, k, m * 128:(m + 1) * 128], mx[:, k, 0:n],
                                                  start=(k == 0), stop=(k == KC - 1)), reads=[Wo, mx], writes=[p])
                S.op("dve", lambda e: e.scalar_tensor_tensor(h[:, m, 0:n], p[:, 0:n], self.mod[:, 16 + m, col:col + 1],
                                                             h[:, m, 0:n], ALU.mult, ALU.add),
                     reads=[p, self.mod, h], writes=[h])
            S.dma("sp", self.Hs.t[:, :, t0:t0 + n], h[:, :, 0:n], reads=[h], writes=[self.Hs])
        S.barrier()
        st.close()

    def even_mixer(self, je, src):
        self.even_proj(je, src)
        self.even_attn(je)
        self.even_pool(je)
        self.mix_out(self.ev_wout.t[je], self.ev_wout, src)

    def odd_proj(self, jo, src):
        S = self.S
        st = ExitStack()
        self.nm_alloc(st)
        W = S.sbuf("odW", [128, KC, 2976], BF16, st)
        for k in range(KC):
            S.dma("pool", W[:, k, :], self.od_w.t[jo, k * 128:(k + 1) * 128, :], reads=[self.od_w], writes=[W])
        mu = S.sbuf("odmu", [128, 22, 2], F32, st)
        S.dma("sp", mu[:], self.mu_l.t[jo], reads=[self.mu_l], writes=[mu])
        c0 = S.sbuf("odc0", [128, 22], F32, st)
        S.op("dve", lambda e: e.tensor_tensor(c0[:], mu[:, :, 0], mu[:, :, 1], ALU.add), reads=[mu], writes=[c0])
        S.op("dve", lambda e: e.tensor_scalar(c0[:], c0[:], -1.0, 1.0, ALU.mult, ALU.add), reads=[c0], writes=[c0])
        ax = S.sbuf("odax", [128, KC, SEQ], BF16, st)
        hb = [S.sbuf(f"odh{i}", [128, KC, 512], F32, st) for i in range(2)]
        ub = [S.sbuf(f"odu{i}", [128, SEQ + 2], F32, st) for i in range(2)]
        t1 = S.sbuf("odt1", [128, SEQ], F32, st)
        fo = [S.sbuf(f"odf{i}", [128, SEQ], BF16, st) for i in range(2)]
        for u in ub:
            S.op("pool", lambda e: e.memset(u[:, 0:1], 0.0), writes=[u])
        tiles = token_tiles()
        ci = 0
        for b in range(NB):
            for isc in (True, False):
                T = CTX if isc else SEQ
                s0 = b * NTB + (0 if isc else CTX)
                segt = [t for t in tiles if t[3] == b and t[4] == isc]
                for ti, (t0, n, col, _, _) in enumerate(segt):
                    h = hb[ti % 2]
                    S.dma("sp", h[:, :, 0:n], src.t[:, :, t0:t0 + n], reads=[src], writes=[h])
                    self.norm_mod_tile(0, h, t0, n, col, ax, ooff=t0 - s0)
                for m in range(24):
                    off = m * 128 if m < 21 else (2688 if m == 21 else 2720 + (m - 22) * 128)
                    mw = 32 if m == 21 else 128
                    u = ub[ci % 2]
                    f = fo[ci % 2]
                    ci += 1
                    for n0 in range(0, T, 512):
                        n = min(512, T - n0)
                        p = self.next_ps()
                        for k in range(KC):
                            S.op("pe", lambda e: e.matmul(p[0:mw, 0:n], W[:, k, off:off + mw], ax[:, k, n0:n0 + n],
                                                          start=(k == 0), stop=(k == KC - 1)), reads=[W, ax], writes=[p])
                        if m >= 22:
                            S.op("act", lambda e: e.activation(f[:, n0:n0 + n], p[:, 0:n], ACT.Identity), reads=[p], writes=[f])
                        else:
                            S.op("act", lambda e: e.activation(u[0:mw, 1 + n0:1 + n0 + n], p[0:mw, 0:n], ACT.Identity),
                                 reads=[p], writes=[u])
                    if m >= 22:
                        S.dma("sp", self.FN.t[:, m - 22, s0:s0 + T], f[:, 0:T], reads=[f], writes=[self.FN])
                        continue
                    S.op("pool", lambda e: e.memset(u[:, T + 1:T + 2], 0.0), writes=[u])
                    S.op("dve", lambda e: e.tensor_scalar(t1[0:mw, 0:T], u[0:mw, 1:T + 1], c0[0:mw, m:m + 1], None, ALU.mult),
                         reads=[u, c0], writes=[t1])
                    S.op("dve", lambda e: e.scalar_tensor_tensor(t1[0:mw, 0:T], u[0:mw, 0:T], mu[0:mw, m, 0:1], t1[0:mw, 0:T],
                                                                 ALU.mult, ALU.add), reads=[u, mu, t1], writes=[t1])
                    S.op("dve", lambda e: e.scalar_tensor_tensor(f[0:mw, 0:T], u[0:mw, 2:T + 2], mu[0:mw, m, 1:2], t1[0:mw, 0:T],
                                                                 ALU.mult, ALU.add), reads=[u, mu, t1], writes=[f])
                    S.dma("sp", self.F.t[0:mw, m, s0:s0 + T], f[0:mw, 0:T], reads=[f], writes=[self.F])
        S.barrier()
        st.close()

    def odd_rwkv(self, jo):
        S = self.S
        st = ExitStack()
        N = NTB
        NTL = N // 128
        CE = float(np.exp(-0.5))
        sb = lambda nm, shp, dt: S.sbuf(nm, shp, dt, st)
        idb = sb("rw_idb", [128, 128], BF16)
        S.op("dve", lambda e: e.tensor_copy(idb[:], self.ident[:]), reads=[self.ident], writes=[idb])
        bo_bf = sb("rw_bo", [128, 128], BF16)
        bo_f = sb("rw_bof", [128, 128], F32)
        S.dma("sp", bo_f[:], self.blk1.t[:, :], reads=[self.blk1], writes=[bo_f])
        S.op("dve", lambda e: e.tensor_copy(bo_bf[:], bo_f[:]), reads=[bo_f], writes=[bo_bf])
        bo64 = sb("rw_bo64", [128, 128], F32)
        S.op("dve", lambda e: e.tensor_scalar(bo64[:], bo_f[:], 1.0 / 64, None, ALU.mult), reads=[bo_f], writes=[bo64])
        msk = sb("rw_msk", [128, 4, 128], F32)
        S.dma("sp", msk[:], self.rmask.t.rearrange("v p n -> p v n"), reads=[self.rmask], writes=[msk])
        nmsk = sb("rw_nmsk", [128, 4, 128], F32)
        S.op("dve", lambda e: e.tensor_scalar(nmsk[:], msk[:], -1.0, None, ALU.mult), reads=[msk], writes=[nmsk])
        w2s = sb("rw_w2", [128, 768], BF16)
        a2s = sb("rw_a2", [128, 768], BF16)
        g2a = sb("rw_g2a", [128, 768], BF16)
        g2b = sb("rw_g2b", [32, 768], BF16)
        S.dma("pool", w2s[:], self.rw_w2.t[jo], reads=[self.rw_w2], writes=[w2s])
        S.dma("pool", a2s[:], self.rw_a2.t[jo], reads=[self.rw_a2], writes=[a2s])
        S.dma("pool", g2a[:], self.rw_g2.t[jo, 0:128, :], reads=[self.rw_g2], writes=[g2a])
        S.dma("pool", g2b[:], self.rw_g2.t[jo, 128:160, :], reads=[self.rw_g2], writes=[g2b])
        pv = sb("rw_pv", [128, 6, 10], F32)
        S.dma("sp", pv[:, :, 0:9], self.rw_pv.t[jo], reads=[self.rw_pv], writes=[pv])
        S.op("dve", lambda e: e.tensor_scalar(pv[:, :, 9], pv[:, :, 5], -1.0, 1.0, ALU.mult, ALU.add), reads=[pv], writes=[pv])
        ones = sb("rw_ones", [128, N], BF16)
        S.op("pool", lambda e: e.memset(ones[:], 1.0), writes=[ones])
        r_f = sb("rw_r", [128, N], BF16)
        k_f = sb("rw_k", [128, N], BF16)
        v_f = sb("rw_v", [128, N], BF16)
        dlc = sb("rw_dl", [128, N], BF16)
        alc = sb("rw_al", [128, N], BF16)
        gl0 = sb("rw_gl0", [128, N], BF16)
        gl1 = sb("rw_gl1", [32, N], BF16)
        th = dlc
        kk = sb("rw_kk", [128, N], BF16)
        sg = sb("rw_sg", [128, N], F32)
        A_ = sb("rw_A", [128, N], F32)
        Asum = sb("rw_Asum", [128, N], F32)
        cum = sb("rw_cum", [128, N], F32)
        kd = sb("rw_kd", [128, N], BF16)
        bq = sb("rw_bq", [128, N], BF16)
        rt = sb("rw_rt", [128, N], BF16)
        P_f = sb("rw_Pf", [128, N], BF16)
        Q_t = sb("rw_Qt", [128, NTL, 2, 64], BF16)
        Kb_t = sb("rw_Kbt", [128, NTL, 128], BF16)
        NBb_t = sb("rw_NBbt", [128, NTL, 128], BF16)
        V_t = sb("rw_Vt", [128, NTL, 128], BF16)
        ArkT = [sb(f"rw_ArkT{i}", [128, NTL, 128], BF16) for i in range(2)]
        NArbT = [sb(f"rw_NArbT{i}", [128, NTL, 128], BF16) for i in range(2)]
        WL = sb("rw_WL", [128, 2 * NTL], F32)
        Yacc = sb("rw_Y", [128, N], F32)
        Mst = sb("rw_M", [128, 64], F32)
        Mb_all = sb("rw_Mball", [128, 2 * NTL, 64], BF16)
        GpT = sb("rw_GpT", [128, 2 * NTL, 64], BF16)
        Hs_ = sb("rw_Hs", [128, 2 * NTL, 64], F32)
        T1 = [[sb(f"rw_T1{i}_{j}", [128, 64], F32) for j in range(2)] for i in range(2)]
        Mstv = [Res("Mst0", Mst.t), Res("Mst1", Mst.t)]
        Mbv = [Res("Mb0", Mb_all.t), Res("Mb1", Mb_all.t)]
        GpTv = [Res("GpT0", GpT.t), Res("GpT1", GpT.t)]
        Hsv = [Res("Hs0", Hs_.t), Res("Hs1", Hs_.t)]
        Yv = [Res("Y0", Yacc.t), Res("Y1", Yacc.t)]
        PT = [[sb(f"rw_PT{i}_{j}", [128, 64], BF16) for j in range(2)] for i in range(2)]
        Qv = [Res("Q0", Q_t.t), Res("Q1", Q_t.t)]
        E = [sb(f"rw_E{i}", [128, 128], F32) for i in range(2)]
        Ep = [sb(f"rw_Ep{i}", [128, 128], F32) for i in range(2)]
        ex = [[sb(f"rw_ex{i}_{j}", [128, 128], F32) for j in range(4)] for i in range(2)]
        kt = [sb(f"rw_kt{i}", [128, 128], BF16) for i in range(2)]
        bt = [sb(f"rw_bt{i}", [128, 128], BF16) for i in range(2)]
        kkh = [sb(f"rw_kkh{i}", [128, 128], BF16) for i in range(2)]
        kbar = [sb(f"rw_kbar{i}", [128, 128], BF16) for i in range(2)]
        nbbar = [sb(f"rw_nbbar{i}", [128, 128], BF16) for i in range(2)]
        KKt = [sb(f"rw_KKt{i}", [128, 128], BF16) for i in range(2)]
        mats = [[[sb(f"rw_mat{a}_{i}_{j}", [128, 128], BF16) for j in range(8)] for i in range(2)] for a in range(2)]
        AVt = [[sb(f"rw_AVt{a}_{i}", [128, 64], BF16) for i in range(2)] for a in range(2)]
        big = [sb(f"rw_big{i}", [128, 512], F32) for i in range(4)]
        bigb = [sb(f"rw_bigb{i}", [128, 512], BF16) for i in range(2)]
        sgl0 = gl0
        sgl1 = gl1
        mo = [sb(f"rw_mo{i}", [128, 512], BF16) for i in range(2)]
        evi = [0]

        def evac(dst_ap, dst_res, src_ap, src_res):
            evi[0] += 1
            if evi[0] % 4 != 0:
                S.op("act", lambda e: e.activation(dst_ap, src_ap, ACT.Identity), reads=[src_res], writes=[dst_res])
            else:
                S.op("dve", lambda e: e.tensor_copy(dst_ap, src_ap), reads=[src_res], writes=[dst_res])

        def mm(p, pap, lhsT, lres, rhs, rres, start=True, stop=True):
            S.op("pe", lambda e: e.matmul(pap, lhsT, rhs, start=start, stop=stop), reads=[lres, rres], writes=[p])

        stage = self.dbg.get("rw_stage", 9)

        class _Stop(Exception):
            pass
        try:
          self._rwkv_body(locals(), _Stop)
        except _Stop:
            pass
        S.barrier()
        st.close()

    def _rwkv_body(self, L, _Stop):
        globals_ = L
        S = self.S
        (N, NTL, CE, idb, bo_bf, bo_f, bo64, msk, nmsk, w2s, a2s, g2a, g2b, pv, ones, r_f, k_f, v_f, dlc, alc, gl0, gl1, th, kk, sg,
         A_, Asum, cum, kd, bq, rt, P_f, Q_t, Kb_t, NBb_t, V_t, ArkT, NArbT, WL, Yacc, Mst, Mb_all, GpT, Hs_, T1, Mstv, Mbv, GpTv, Hsv, Yv, PT, Qv,
         E, Ep, ex, kt, bt, kkh, kbar,
         nbbar, KKt, mats, AVt, big, bigb, sgl0, sgl1, mo, evac, mm, stage) = [L[k] for k in (
            "N NTL CE idb bo_bf bo_f bo64 msk nmsk w2s a2s g2a g2b pv ones r_f k_f v_f dlc alc gl0 gl1 th kk sg "
            "A_ Asum cum kd bq rt P_f Q_t Kb_t NBb_t V_t ArkT NArbT WL Yacc Mst Mb_all GpT Hs_ T1 Mstv Mbv GpTv Hsv Yv PT Qv E Ep ex kt bt kkh kbar "
            "nbbar KKt mats AVt big bigb sgl0 sgl1 mo evac mm stage").split()]
        for b in range(NB):
            b0 = b * NTB
            S.dma("sp", dlc[:], self.F.t[:, 18, b0:b0 + N], reads=[self.F], writes=[dlc])
            S.dma("sp", alc[:], self.F.t[:, 19, b0:b0 + N], reads=[self.F], writes=[alc])
            S.dma("sp", gl0[:], self.F.t[:, 20, b0:b0 + N], reads=[self.F], writes=[gl0])
            S.dma("sp", gl1[:], self.F.t[0:32, 21, b0:b0 + N], reads=[self.F], writes=[gl1])
            S.op("act", lambda e: e.activation(th[:], dlc[:], ACT.Tanh), reads=[dlc], writes=[dlc])
            S.op("act", lambda e: e.activation(sgl0[:], gl0[:], ACT.Sigmoid), reads=[gl0], writes=[gl0])
            S.op("act", lambda e: e.activation(sgl1[:], gl1[:], ACT.Sigmoid), reads=[gl1], writes=[gl1])
            for hp in range(6):
                S.dma("sp", r_f[:], self.F.t[:, hp, b0:b0 + N], reads=[self.F], writes=[r_f])
                S.dma("sp", k_f[:], self.F.t[:, 6 + hp, b0:b0 + N], reads=[self.F], writes=[k_f])
                S.dma("sp", v_f[:], self.F.t[:, 12 + hp, b0:b0 + N], reads=[self.F], writes=[v_f])
                S.op("dve", lambda e: e.tensor_scalar(kk[:], k_f[:], pv[:, hp, 4:5], None, ALU.mult), reads=[k_f, pv], writes=[kk])
                for n0 in range(0, N, 512):
                    n = min(512, N - n0)
                    sq = bigb[(n0 // 512) % 2]
                    S.op("act", lambda e: e.activation(sq[:, 0:n], kk[:, n0:n0 + n], ACT.Square), reads=[kk], writes=[sq])
                    p = self.next_ps()
                    mm(p, p[:, 0:n], bo_bf[:], bo_bf, sq[:, 0:n], sq)
                    nr = big[(n0 // 512) % 2]
                    S.op("act", lambda e: e.activation(nr[:, 0:n], p[:, 0:n], ACT.Sqrt), reads=[p], writes=[nr])
                    S.op("dve", lambda e: e.tensor_scalar(nr[:, 0:n], nr[:, 0:n], 1e-12, None, ALU.max), reads=[nr], writes=[nr])
                    S.op("dve", lambda e: e.reciprocal(nr[:, 0:n], nr[:, 0:n]), reads=[nr], writes=[nr])
                    S.op("dve", lambda e: e.tensor_tensor(kk[:, n0:n0 + n], kk[:, n0:n0 + n], nr[:, 0:n], ALU.mult),
                         reads=[kk, nr], writes=[kk])
                if stage <= 0:
                    raise _Stop()
                for tl in range(NTL):
                    p = self.next_ps()
                    mm(p, p[:, 0:128], v_f[:, tl * 128:(tl + 1) * 128], v_f, idb[:], idb)
                    evac(V_t[:, tl, :], V_t, p[:, 0:128], p)
                for d in range(2):
                    ds = slice(d * 64, (d + 1) * 64)
                    for n0 in range(0, N, 512):
                        n = min(512, N - n0)
                        p = self.next_ps()
                        mm(p, p[:, 0:n], w2s[ds, hp * 128:(hp + 1) * 128], w2s, th[ds, n0:n0 + n], th)
                        S.op("act", lambda e: e.activation(sg[:, n0:n0 + n], p[:, 0:n], ACT.Sigmoid, bias=pv[:, hp, d:d + 1]),
                             reads=[p, pv], writes=[sg])
                        p = self.next_ps()
                        mm(p, p[:, 0:n], a2s[ds, hp * 128:(hp + 1) * 128], a2s, alc[ds, n0:n0 + n], alc)
                        S.op("act", lambda e: e.activation(A_[:, n0:n0 + n], p[:, 0:n], ACT.Sigmoid, bias=pv[:, hp, 2 + d:3 + d]),
                             reads=[p, pv], writes=[A_])
                    if d == 0:
                        S.op("pool", lambda e: e.tensor_copy(Asum[:], A_[:]), reads=[A_], writes=[Asum])
                    else:
                        S.op("pool", lambda e: e.tensor_tensor(Asum[:], Asum[:], A_[:], ALU.add), reads=[A_, Asum], writes=[Asum])
                    S.op("dve", lambda e: e.tensor_tensor_scan(cum[:], ones[:], sg[:], 0.0, ALU.mult, ALU.add),
                         reads=[ones, sg], writes=[cum])
                    S.op("dve", lambda e: e.tensor_scalar(kd[:], A_[:], pv[:, hp, 5:6], pv[:, hp, 9:10], ALU.mult, ALU.add),
                         reads=[A_, pv], writes=[kd])
                    S.op("dve", lambda e: e.tensor_tensor(kd[:], kd[:], k_f[:], ALU.mult), reads=[kd, k_f], writes=[kd])
                    S.op("pool", lambda e: e.tensor_tensor(bq[:], kk[:], A_[:], ALU.mult), reads=[kk, A_], writes=[bq])
                    if stage <= 1:
                        raise _Stop()
                    ms, mi = (0, 1) if d == 0 else (2, 3)
                    msT = 2 if d == 0 else 0
                    pending = []
                    for tl in range(NTL):
                        i2 = tl % 2
                        ts_ = slice(tl * 128, (tl + 1) * 128)
                        e_, ep_ = E[i2], Ep[i2]
                        eW, eWi, eWp, eWl = ex[i2]
                        for half in range(2):
                            cc = 2 * tl + half
                            c_ = slice(cc * 64, (cc + 1) * 64)
                            l_ = slice(half * 64, (half + 1) * 64)
                            if d == 0:
                                if cc == 0:
                                    S.op("dve", lambda e: e.tensor_scalar(e_[:, l_], cum[:, c_], -CE, None, ALU.mult),
                                         reads=[cum], writes=[e_])
                                else:
                                    S.op("dve", lambda e: e.tensor_scalar(e_[:, l_], cum[:, c_], cum[:, cc * 64 - 1:cc * 64], -CE,
                                                                          ALU.subtract, ALU.mult), reads=[cum], writes=[e_])
                                S.op("dve", lambda e: e.scalar_tensor_tensor(ep_[:, l_], sg[:, c_], CE, e_[:, l_], ALU.mult, ALU.add),
                                     reads=[sg, e_], writes=[ep_])
                                tot = e_[:, half * 64 + 63:half * 64 + 64]
                            else:
                                S.op("dve", lambda e: e.tensor_scalar(ep_[:, l_], cum[:, c_], cum[:, cc * 64 + 63:cc * 64 + 64], CE,
                                                                      ALU.subtract, ALU.mult), reads=[cum], writes=[ep_])
                                S.op("dve", lambda e: e.scalar_tensor_tensor(e_[:, l_], sg[:, c_], -CE, ep_[:, l_], ALU.mult, ALU.add),
                                     reads=[sg, ep_], writes=[e_])
                                tot = e_[:, half * 64:half * 64 + 1]
                            S.op("act", lambda e: e.activation(eWl[:, l_], e_[:, l_], ACT.Exp, bias=tot, scale=-1.0),
                                 reads=[e_], writes=[eWl])
                            S.op("act", lambda e: e.activation(WL[:, cc:cc + 1], tot, ACT.Exp), reads=[e_], writes=[WL])
                        S.op("act", lambda e: e.activation(eW[:], e_[:], ACT.Exp), reads=[e_], writes=[eW])
                        S.op("act", lambda e: e.activation(eWi[:], e_[:], ACT.Exp, scale=-1.0), reads=[e_], writes=[eWi])
                        S.op("act", lambda e: e.activation(eWp[:], ep_[:], ACT.Exp), reads=[ep_], writes=[eWp])
                        S.op("dve", lambda e: e.tensor_tensor(rt[:, ts_], r_f[:, ts_], eW[:], ALU.mult), reads=[r_f, eW], writes=[rt])
                        S.op("pool", lambda e: e.tensor_tensor(kt[i2][:], kd[:, ts_], eWi[:], ALU.mult), reads=[kd, eWi], writes=[kt[i2]])
                        S.op("dve", lambda e: e.tensor_tensor(bt[i2][:], bq[:, ts_], eWi[:], ALU.mult), reads=[bq, eWi], writes=[bt[i2]])
                        S.op("pool", lambda e: e.tensor_tensor(kkh[i2][:], kk[:, ts_], eWp[:], ALU.mult), reads=[kk, eWp], writes=[kkh[i2]])
                        S.op("dve", lambda e: e.tensor_tensor(kbar[i2][:], kd[:, ts_], eWl[:], ALU.mult), reads=[kd, eWl], writes=[kbar[i2]])
                        S.op("dve", lambda e: e.scalar_tensor_tensor(nbbar[i2][:], bq[:, ts_], -1.0, eWl[:], ALU.mult, ALU.mult),
                             reads=[bq, eWl], writes=[nbbar[i2]])
                        if stage == 2 and self.dbg.get("rw_sub", 9) <= 0:
                            continue
                        for (src_, dst_ap, dst_r) in ((kkh[i2], KKt[i2][:], KKt[i2]), (kbar[i2], Kb_t[:, tl, :], Kb_t),
                                                      (nbbar[i2], NBb_t[:, tl, :], NBb_t)):
                            p = self.next_ps()
                            mm(p, p[:, 0:128], src_[:], src_, idb[:], idb)
                            evac(dst_ap, dst_r, p[:, 0:128], p)
                        def head_gen(tl=tl, i2=i2, ts_=ts_):
                            def one(hh):
                                hs = slice(hh * 64, (hh + 1) * 64)
                                X, XT, Y, YT, R, AkkT_, Y2, YT2 = mats[i2][hh]

                                def masked(dst_ap, dst_r, lhs, rhs_ap, rhs_r, mk, neg=False):
                                    p = self.next_ps()
                                    mm(p, p[:, 0:128], lhs[hs, :], lhs, rhs_ap, rhs_r)
                                    mres = nmsk if neg else msk
                                    S.op("dve", lambda e: e.tensor_tensor(dst_ap, p[:, 0:128], mres[:, mk, :], ALU.mult),
                                         reads=[p, mres], writes=[dst_r])
                                masked(X[:], X, bt[i2], kkh[i2][hs, :], kkh[i2], ms)
                                masked(XT[:], XT, kkh[i2], bt[i2][hs, :], bt[i2], msT)
                                yield
                                masked(AkkT_[:], AkkT_, kt[i2], kkh[i2][hs, :], kkh[i2], ms)
                                masked(ArkT[hh][:, tl, :], ArkT[hh], kt[i2], rt[hs, ts_], rt, mi)
                                masked(NArbT[hh][:, tl, :], NArbT[hh], bt[i2], rt[hs, ts_], rt, mi, neg=True)
                                S.op("dve", lambda e: e.tensor_tensor(R[:], idb[:], X[:], ALU.subtract), reads=[idb, X], writes=[R])
                                p = self.next_ps()
                                mm(p, p[:, 0:64], AkkT_[:], AkkT_, V_t[:, tl, hs], V_t)
                                evac(AVt[i2][hh][:], AVt[i2][hh], p[:, 0:64], p)
                                yield
                                cy, cyt, ny, nyt = X, XT, Y, YT
                                for lvl in range(5):
                                    if lvl < 4:
                                        p = self.next_ps()
                                        mm(p, p[:, 0:128], cyt[:], cyt, cy[:], cy)
                                        evac(ny[:], ny, p[:, 0:128], p)
                                    p = self.next_ps()
                                    mm(p, p[:, 0:128], cy[:], cy, cyt[:], cyt)
                                    evac(nyt[:], nyt, p[:, 0:128], p)
                                    yield
                                    p = self.next_ps()
                                    mm(p, p[:, 0:128], nyt[:], nyt, R[:], R)
                                    S.op("dve", lambda e: e.tensor_tensor(R[:], p[:, 0:128], R[:], ALU.add), reads=[p, R], writes=[R])
                                    cy, cyt = ny, nyt
                                    ny, nyt = (Y2, YT2) if ny is Y else (Y, YT)
                                    yield
                                p = self.next_ps()
                                mm(p, p[hs, 0:128], KKt[i2][:, hs], KKt[i2], R[:], R)
                                evac(P_f[hs, ts_], P_f, p[hs, 0:128], p)
                                p = self.next_ps()
                                mm(p, p[:, 0:64], R[:], R, KKt[i2][:, hs], KKt[i2])
                                evac(PT[i2][hh][:], PT[i2][hh], p[:, 0:64], p)
                                p = self.next_ps()
                                mm(p, p[:, 0:64], R[:], R, AVt[i2][hh][:], AVt[i2][hh])
                                evac(Q_t[:, tl, hh, :], Qv[hh], p[:, 0:64], p)
                                yield
                                for half in range(2):
                                    cc = 2 * tl + half
                                    cs = slice(half * 64, (half + 1) * 64)
                                    pg = self.next_ps()
                                    mm(pg, pg[hs, 0:64], PT[i2][hh][cs, :], PT[i2][hh], NBb_t[cs, tl, hs], NBb_t)
                                    evac(GpT[hs, cc, :], GpTv[hh], pg[hs, 0:64], pg)
                                    ph = self.next_ps()
                                    mm(ph, ph[hs, 0:64], Kb_t[cs, tl, hs], Kb_t, V_t[cs, tl, hs], V_t, start=True, stop=False)
                                    mm(ph, ph[hs, 0:64], NBb_t[cs, tl, hs], NBb_t, Q_t[cs, tl, hh, :], Qv[hh], start=False, stop=True)
                                    evac(Hs_[hs, cc, :], Hsv[hh], ph[hs, 0:64], ph)
                            return [one(0), one(1)]
                        pending.extend(head_gen())
                        if tl % 2 == 1 or tl == NTL - 1:
                            while pending:
                                for g_ in list(pending):
                                    try:
                                        next(g_)
                                    except StopIteration:
                                        pending.remove(g_)
                    if stage <= 2:
                        raise _Stop()
                    if d == 0:
                        order = list(range(2 * NTL))
                    else:
                        order = list(range(CTX // 64 - 1, -1, -1)) + list(range(2 * NTL - 1, CTX // 64 - 1, -1))
                    S.op("pool", lambda e: e.memset(Mst[:], 0.0), writes=[Mstv[0], Mstv[1]])
                    S.op("pool", lambda e: e.memset(Mb_all[:, order[0], :], 0.0), writes=[Mbv[0], Mbv[1]])
                    def chain_step(oi):
                        cc = order[oi]
                        ncc = order[oi + 1] if oi + 1 < len(order) else None
                        for hh in range(2):
                            hs = slice(hh * 64, (hh + 1) * 64)
                            t1 = T1[hh][oi % 2]
                            S.op("dve", lambda e: e.scalar_tensor_tensor(t1[hs, :], Mst[hs, :], WL[hs, cc:cc + 1], Hs_[hs, cc, :],
                                                                         ALU.mult, ALU.add),
                                 reads=[Mstv[hh], WL, Hsv[hh]], writes=[t1])
                            p = self.next_ps()
                            mm(p, p[hs, 0:64], GpT[hs, cc, :], GpTv[hh], Mb_all[hs, cc, :], Mbv[hh])
                            if ncc is not None:
                                S.op("dve", lambda e: e.tensor_tensor(Mb_all[hs, ncc, :], p[hs, 0:64], t1[hs, :], ALU.add),
                                     reads=[p, t1], writes=[Mbv[hh]])
                                S.op("dve", lambda e: e.tensor_tensor(Mst[hs, :], p[hs, 0:64], t1[hs, :], ALU.add),
                                     reads=[p, t1], writes=[Mstv[hh]])

                    def bulk_u(cc):
                        tl, half = cc // 2, cc % 2
                        cs = slice(half * 64, (half + 1) * 64)
                        c_ = slice(cc * 64, (cc + 1) * 64)
                        for hh in range(2):
                            hs = slice(hh * 64, (hh + 1) * 64)
                            pu = self.next_ps()
                            mm(pu, pu[cs, 0:64], P_f[hs, c_], P_f, Mb_all[hs, cc, :], Mbv[hh])
                            S.op("dve", lambda e: e.tensor_tensor(Q_t[cs, tl, hh, :], pu[cs, 0:64], Q_t[cs, tl, hh, :], ALU.add),
                                 reads=[pu, Qv[hh]], writes=[Qv[hh]])

                    def bulk_y(cc):
                        tl, half = cc // 2, cc % 2
                        cs = slice(half * 64, (half + 1) * 64)
                        c_ = slice(cc * 64, (cc + 1) * 64)
                        for hh in range(2):
                            hs = slice(hh * 64, (hh + 1) * 64)
                            py = self.next_ps()
                            mm(py, py[hs, 0:64], Mb_all[hs, cc, :], Mbv[hh], rt[hs, c_], rt, start=True, stop=True)
                            py2 = self.next_ps()
                            mm(py2, py2[hs, 0:64], V_t[cs, tl, hs], V_t, ArkT[hh][cs, tl, cs], ArkT[hh], start=True, stop=False)
                            mm(py2, py2[hs, 0:64], Q_t[cs, tl, hh, :], Qv[hh], NArbT[hh][cs, tl, cs], NArbT[hh], start=False, stop=True)
                            if d == 0:
                                S.op("act", lambda e: e.activation(Yacc[hs, c_], py[hs, 0:64], ACT.Identity), reads=[py], writes=[Yv[hh]])
                            else:
                                S.op("dve", lambda e: e.tensor_tensor(Yacc[hs, c_], py[hs, 0:64], Yacc[hs, c_], ALU.add),
                                     reads=[py, Yv[hh]], writes=[Yv[hh]])
                            S.op("dve", lambda e: e.tensor_tensor(Yacc[hs, c_], py2[hs, 0:64], Yacc[hs, c_], ALU.add),
                                 reads=[py2, Yv[hh]], writes=[Yv[hh]])

                    nord = len(order)
                    for oi in range(nord + 2):
                        if oi < nord:
                            chain_step(oi)
                        if 1 <= oi <= nord:
                            bulk_u(order[oi - 1])
                        if oi >= 2:
                            bulk_y(order[oi - 2])
                if stage <= 3:
                    raise _Stop()
                for n0 in range(0, N, 512):
                    n = min(512, N - n0)
                    ns = slice(n0, n0 + n)
                    yc, sq_, yn, cf = big
                    p = self.next_ps()
                    mm(p, p[:, 0:n], bo64[:], bo64, Yacc[:, ns], Yv[0])
                    S.op("dve", lambda e: e.tensor_tensor(yc[:, 0:n], Yacc[:, ns], p[:, 0:n], ALU.subtract), reads=[Yv[0], Yv[1], p], writes=[yc])
                    S.op("act", lambda e: e.activation(sq_[:, 0:n], yc[:, 0:n], ACT.Square), reads=[yc], writes=[sq_])
                    p = self.next_ps()
                    mm(p, p[:, 0:n], bo64[:], bo64, sq_[:, 0:n], sq_)
                    S.op("act", lambda e: e.activation(sq_[:, 0:n], p[:, 0:n], ACT.Sqrt, bias=self.gneps[:, 0:1]), reads=[p, self.gneps], writes=[sq_])
                    S.op("dve", lambda e: e.reciprocal(sq_[:, 0:n], sq_[:, 0:n]), reads=[sq_], writes=[sq_])
                    S.op("dve", lambda e: e.tensor_tensor(yn[:, 0:n], yc[:, 0:n], sq_[:, 0:n], ALU.mult), reads=[yc, sq_], writes=[yn])
                    S.op("dve", lambda e: e.tensor_scalar(yn[:, 0:n], yn[:, 0:n], pv[:, hp, 7:8], pv[:, hp, 8:9], ALU.mult, ALU.add),
                         reads=[yn, pv], writes=[yn])
                    S.op("dve", lambda e: e.tensor_scalar(cf[:, 0:n], Asum[:, ns], pv[:, hp, 5:6], pv[:, hp, 9:10], ALU.mult, ALU.add),
                         reads=[Asum, pv], writes=[cf])
                    S.op("dve", lambda e: e.tensor_scalar(cf[:, 0:n], cf[:, 0:n], pv[:, hp, 9:10], None, ALU.add),
                         reads=[cf, pv], writes=[cf])
                    S.op("dve", lambda e: e.tensor_tensor(cf[:, 0:n], cf[:, 0:n], k_f[:, ns], ALU.mult), reads=[cf, k_f], writes=[cf])
                    pb = bigb[0]
                    S.op("dve", lambda e: e.scalar_tensor_tensor(pb[:, 0:n], cf[:, 0:n], pv[:, hp, 6:7], r_f[:, ns], ALU.mult, ALU.mult),
                         reads=[cf, pv, r_f], writes=[pb])
                    p = self.next_ps()
                    mm(p, p[:, 0:n], bo_bf[:], bo_bf, pb[:, 0:n], pb)
                    S.op("dve", lambda e: e.tensor_tensor(cf[:, 0:n], p[:, 0:n], v_f[:, ns], ALU.mult), reads=[p, v_f], writes=[cf])
                    S.op("pool", lambda e: e.tensor_tensor(yn[:, 0:n], yn[:, 0:n], cf[:, 0:n], ALU.add), reads=[yn, cf], writes=[yn])
                    p = self.next_ps()
                    mm(p, p[:, 0:n], g2a[:, hp * 128:(hp + 1) * 128], g2a, sgl0[:, ns], sgl0, start=True, stop=False)
                    mm(p, p[:, 0:n], g2b[:, hp * 128:(hp + 1) * 128], g2b, sgl1[:, ns], sgl1, start=False, stop=True)
                    m_ = mo[(n0 // 512) % 2]
                    S.op("dve", lambda e: e.tensor_tensor(m_[:, 0:n], p[:, 0:n], yn[:, 0:n], ALU.mult), reads=[p, yn], writes=[m_])
                    S.dma("sp", self.MIX.t[:, hp, b0 + n0:b0 + n0 + n], m_[:, 0:n], reads=[m_], writes=[self.MIX])

    def odd_fnet(self, jo):
        S = self.S
        st = ExitStack()
        CB = S.sbuf("fnCB", [128, 128], BF16, st)
        SB = S.sbuf("fnSB", [128, 128], BF16, st)
        S.dma("sp", CB[:], self.dft64.t[0], reads=[self.dft64], writes=[CB])
        S.dma("sp", SB[:], self.dft64.t[1], reads=[self.dft64], writes=[SB])
        CT = S.sbuf("fnCT", [128, SEQ // 128, SEQ], BF16, st)
        NST = S.sbuf("fnNST", [128, SEQ // 128, SEQ], BF16, st)
        u = S.sbuf("fnu", [128, 2, SEQ], BF16, st)
        AC = S.sbuf("fnAC", [128, SEQ // 128, 2, 128], BF16, st)
        AS = S.sbuf("fnAS", [128, SEQ // 128, 2, 128], BF16, st)
        mo = [S.sbuf(f"fnmo{i}", [128, 512], BF16, st) for i in range(2)]
        oi = 0
        for isc in (True, False):
            T = CTX if isc else SEQ
            NTT = T // 128
            tab = self.dftc if isc else self.dftx
            for tl in range(NTT):
                S.dma("sp", CT[:, tl, 0:T], tab.t[0, :, tl, :], reads=[tab], writes=[CT])
                S.dma("sp", NST[:, tl, 0:T], tab.t[1, :, tl, :], reads=[tab], writes=[NST])
            for b in range(NB):
                s0 = b * NTB + (0 if isc else CTX)
                S.dma("sp", u[:, :, 0:T], self.FN.t[:, :, s0:s0 + T], reads=[self.FN], writes=[u])
                for tl in range(NTT):
                    for ch in range(2):
                        for (dst, tb) in ((AC, CB), (AS, SB)):
                            p = self.next_ps()
                            S.op("pe", lambda e: e.matmul(p[:, 0:128], u[:, ch, tl * 128:(tl + 1) * 128], tb[:], start=True, stop=True),
                                 reads=[u, tb], writes=[p])
                            S.op("act" if dst is AC else "dve",
                                 (lambda e: e.activation(dst[:, tl, ch, :], p[:, 0:128], ACT.Identity)) if dst is AC else
                                 (lambda e: e.tensor_copy(dst[:, tl, ch, :], p[:, 0:128])), reads=[p], writes=[dst])
                for ch in range(2):
                    for n0 in range(0, T, 512):
                        n = min(512, T - n0)
                        p = self.next_ps()
                        for tl in range(NTT):
                            S.op("pe", lambda e: e.matmul(p[:, 0:n], AC[:, tl, ch, :], CT[:, tl, n0:n0 + n], start=(tl == 0), stop=False),
                                 reads=[AC, CT], writes=[p])
                            S.op("pe", lambda e: e.matmul(p[:, 0:n], AS[:, tl, ch, :], NST[:, tl, n0:n0 + n], start=False,
                                                          stop=(tl == NTT - 1)), reads=[AS, NST], writes=[p])
                        m_ = mo[oi % 2]
                        oi += 1
                        S.op("act", lambda e: e.activation(m_[:, 0:n], p[:, 0:n], ACT.Identity), reads=[p], writes=[m_])
                        S.dma("sp", self.MIX.t[:, 6 + ch, s0 + n0:s0 + n0 + n], m_[:, 0:n], reads=[m_], writes=[self.MIX])
        S.barrier()
        st.close()

    def odd_mixer(self, jo, src):
        parts = self.dbg.get("odd", ("proj", "rwkv", "fnet", "out"))
        if "proj" in parts:
            self.odd_proj(jo, src)
        if "rwkv" in parts:
            self.odd_rwkv(jo)
        if "fnet" in parts:
            self.odd_fnet(jo)
        if "out" in parts:
            self.mix_out(self.od_wout.t[jo], self.od_wout, src)

    def moe(self, li, src, groups=None):
        S = self.S
        NG = 1536
        if groups is None:
            groups = [(g * NG, NG) for g in range(NT // NG)]
        st = ExitStack()
        fx = S.sbuf("moe_fx", [128, KC, NG], BF16, st)
        acc = S.sbuf("moe_acc", [128, KC, NG], F32, st)
        hid = S.sbuf("moe_hid", [128, KC, NG], BF16, st)
        gbc = [S.sbuf(f"moe_gbc{i}", [128, NG], BF16, st) for i in range(2)]
        wgu = [S.sbuf(f"moe_wgu{i}", [128, KC, 256], BF16, st) for i in range(3)]
        wst = [S.sbuf(f"moe_wst{i}", [128, KC, 256], F32, st) for i in range(2)]
        si = 0
        wdn = [S.sbuf(f"moe_wdn{i}", [128, KC, 256], BF16, st) for i in range(3)]
        bgu = S.sbuf("moe_bgu", [128, NE, 16], F32, st)
        bdn = S.sbuf("moe_bdn", [NE, D], F32, st)
        sel = S.sbuf("moe_sel", [NE, NE * 128], F32, st)
        t_g = [S.sbuf(f"moe_tg{i}", [128, 512], F32, st) for i in range(2)]
        t_s = [S.sbuf(f"moe_ts{i}", [128, 512], F32, st) for i in range(2)]
        t_l = [S.sbuf(f"moe_tl{i}", [128, 512], F32, st) for i in range(2)]
        hb = [S.sbuf(f"moe_h{i}", [128, KC, 128], F32, st) for i in range(2)]
        S.dma("sp", bgu[:], self.b_gu.t[li], reads=[self.b_gu], writes=[bgu])
        S.dma("sp", bdn[:], self.b_dn.t[li], reads=[self.b_dn], writes=[bdn])
        S.dma("sp", sel[:], self.sel.t[:, :], reads=[self.sel], writes=[sel])
        wi = 0
        di = 0
        ev = 0
        dbg = self.dbg
        for (g0, ng) in groups[:dbg.get("ngrp", len(groups))]:
            ntg = ng // 512
            S.dma("sp", fx[:, :, 0:ng], self.FX.t[:, :, g0:g0 + ng], reads=[self.FX], writes=[fx])
            for e_ in range(dbg.get("ne", NE)):
                gb = gbc[e_ % 2]
                for tg in range(ntg if dbg.get("gate", True) else 0):
                    p = self.next_ps()
                    S.op("pe", lambda e: e.matmul(p[:, :], sel[:, e_ * 128:(e_ + 1) * 128],
                                                  self.gatesT[:, g0 + tg * 512:g0 + (tg + 1) * 512], start=True, stop=True),
                         reads=[sel, self.gatesT], writes=[p])
                    S.op("act", lambda e: e.activation(gb[:, tg * 512:(tg + 1) * 512], p[:, :], ACT.Identity),
                         reads=[p], writes=[gb])
                for j in range(KC if dbg.get("gu", True) else 0):
                    w = wgu[wi % 3]
                    wi += 1
                    ws = wst[si % 2]
                    si += 1
                    S.dma("sp", ws[:], self.w_gu.t[li, e_, :, j * 256:(j + 1) * 256].rearrange("(k p) n -> p k n", p=128),
                          reads=[self.w_gu], writes=[ws])
                    S.op("pool", lambda e: e.tensor_copy(w[:], ws[:]), reads=[ws], writes=[w])
                    for tg in range(ntg):
                        pg = self.next_ps()
                        pl = self.next_ps()
                        for half, pp in ((0, pg), (1, pl)):
                            for k in range(KC):
                                S.op("pe", lambda e: e.matmul(pp[:, :], w[:, k, half * 128:(half + 1) * 128],
                                                              fx[:, k, tg * 512:(tg + 1) * 512],
                                                              start=(k == 0), stop=(k == KC - 1)),
                                     reads=[w, fx], writes=[pp])
                        a = t_g[ev % 2]
                        s_ = t_s[ev % 2]
                        l_ = t_l[ev % 2]
                        ev += 1
                        S.op("dve", lambda e: e.tensor_scalar(a[:], pg[:, :], bgu[:, e_, 2 * j:2 * j + 1], 7.0, ALU.add, ALU.min),
                             reads=[pg, bgu], writes=[a])
                        S.op("act", lambda e: e.activation(s_[:], a[:], ACT.Silu, scale=1.702), reads=[a], writes=[s_])
                        S.op("dve", lambda e: e.tensor_scalar(l_[:], pl[:, :], bgu[:, e_, 2 * j + 1:2 * j + 2], None, ALU.add),
                             reads=[pl, bgu], writes=[l_])
                        S.op("pool", lambda e: e.tensor_scalar(l_[:], l_[:], 7.0, -7.0, ALU.min, ALU.max), reads=[l_], writes=[l_])
                        S.op("dve", lambda e: e.scalar_tensor_tensor(s_[:], l_[:], 1.0, s_[:], ALU.add, ALU.mult),
                             reads=[s_, l_], writes=[s_])
                        S.op("dve", lambda e: e.scalar_tensor_tensor(hid[:, j, tg * 512:(tg + 1) * 512], s_[:], 1.0 / 1.702,
                                                                     gb[:, tg * 512:(tg + 1) * 512], ALU.mult, ALU.mult),
                             reads=[s_, gb], writes=[hid])
                for jo in range(4 if dbg.get("dn", True) else 0):
                    w = wdn[di % 3]
                    di += 1
                    ws = wst[si % 2]
                    si += 1
                    S.dma("sp", ws[:], self.w_dn.t[li, e_, :, jo * 256:(jo + 1) * 256].rearrange("(k p) n -> p k n", p=128),
                          reads=[self.w_dn], writes=[ws])
                    S.op("pool", lambda e: e.tensor_copy(w[:], ws[:]), reads=[ws], writes=[w])
                    for m2 in range(2):
                        oc = jo * 2 + m2
                        for tg in range(ntg):
                            p = self.next_ps()
                            for k in range(KC):
                                S.op("pe", lambda e: e.matmul(p[:, :], w[:, k, m2 * 128:(m2 + 1) * 128],
                                                              hid[:, k, tg * 512:(tg + 1) * 512],
                                                              start=(k == 0), stop=(k == KC - 1)),
                                     reads=[w, hid], writes=[p])
                            dst = acc[:, oc, tg * 512:(tg + 1) * 512]
                            if e_ == 0:
                                S.op("act", lambda e: e.activation(dst, p[:, :], ACT.Identity), reads=[p], writes=[acc])
                            else:
                                S.op("dve", lambda e: e.tensor_tensor(dst, p[:, :], dst, ALU.add), reads=[p, acc], writes=[acc])
            for oc in range(KC if dbg.get("bias", True) else 0):
                for tg in range(ntg):
                    p = self.next_ps()
                    S.op("pe", lambda e: e.matmul(p[:, :], bdn[:, oc * 128:(oc + 1) * 128],
                                                  self.gatesT[:, g0 + tg * 512:g0 + (tg + 1) * 512], start=True, stop=True),
                         reads=[bdn, self.gatesT], writes=[p])
                    dst = acc[:, oc, tg * 512:(tg + 1) * 512]
                    S.op("dve", lambda e: e.tensor_tensor(dst, p[:, :], dst, ALU.add), reads=[p, acc], writes=[acc])
            for bi in range(ng // 128):
                t0 = g0 + bi * 128
                b = t0 // NTB
                col = 2 if (t0 - b * NTB) < CTX else b
                h = hb[bi % 2]
                S.dma("sp", h[:], src.t[:, :, t0:t0 + 128], reads=[src], writes=[h])
                for c in range(KC):
                    S.op("dve", lambda e: e.scalar_tensor_tensor(h[:, c, :], acc[:, c, bi * 128:(bi + 1) * 128],
                                                                 self.mod[:, 40 + c, col:col + 1], h[:, c, :], ALU.mult, ALU.add),
                         reads=[acc, self.mod, h], writes=[h])
                S.dma("sp", self.Hs.t[:, :, t0:t0 + 128], h[:], reads=[h], writes=[self.Hs])
        S.barrier()
        st.close()

    def final_norm(self, src):
        S = self.S
        st = ExitStack()
        hb = [S.sbuf(f"fnh{i}", [128, KC, 512], F32, st) for i in range(2)]
        sq = S.sbuf("fn_sq", [128, KC, 512], BF16, st)
        rs = S.sbuf("fn_rs", [128, 512], F32, st)
        fg = S.sbuf("fn_g", [128, KC], F32, st)
        eps_t = S.sbuf("fn_eps", [128, 1], F32, st)
        S.op("pool", lambda e: e.memset(eps_t[:], EPS), writes=[eps_t])
        S.dma("sp", fg[:], self.final_g.t[:, :], reads=[self.final_g], writes=[fg])
        for ti in range(NT // 512):
            t0 = ti * 512
            h = hb[ti % 2]
            S.dma("sp", h[:], src.t[:, :, t0:t0 + 512], reads=[src], writes=[h])
            S.op("act", lambda e: e.activation(sq[:], h[:], ACT.Square), reads=[h], writes=[sq])
            p = self.next_ps()
            for k in range(KC):
                S.op("pe", lambda e: e.matmul(p[:, :], self.ones_bf[:], sq[:, k, :], start=(k == 0), stop=(k == KC - 1)),
                     reads=[self.ones_bf, sq], writes=[p])
            S.op("act", lambda e: e.activation(rs[:], p[:, :], ACT.Sqrt, bias=eps_t[:, 0:1], scale=1.0 / D),
                 reads=[p, eps_t], writes=[rs])
            S.op("dve", lambda e: e.reciprocal(rs[:], rs[:]), reads=[rs], writes=[rs])
            for c in range(KC):
                S.op("dve", lambda e: e.scalar_tensor_tensor(h[:, c, :], h[:, c, :], fg[:, c:c + 1], rs[:], ALU.mult, ALU.mult),
                     reads=[h, fg, rs], writes=[h])
            S.dma("sp", self.OUT.t[:, :, t0:t0 + 512], h[:], reads=[h], writes=[self.OUT])
        S.barrier()
        st.close()


def fm(a):
    n = a.shape[0]
    return np.ascontiguousarray(a.reshape(n, KC, 128).transpose(2, 1, 0))


def prep_shared(inp, layers, ne=NE):
    L = list(layers)
    o = {}
    o["ada_w"] = np.ascontiguousarray(inp["ada_w"][L])
    ab = inp["ada_b"][L].reshape(len(L), 48, 128).transpose(0, 2, 1)
    o["ada_b4"] = np.ascontiguousarray(np.repeat(ab[..., None], 4, axis=-1))
    ng = np.stack([inp["norm_mix_g"][L], inp["norm_ffn_g"][L]], axis=1)
    o["ng"] = np.ascontiguousarray(ng.reshape(len(L), 2, KC, 128).transpose(0, 1, 3, 2))
    o["router_w"] = np.ascontiguousarray(inp["router_w"][L].reshape(len(L), KC, 128, NE).transpose(0, 2, 1, 3))
    o["router_b"] = np.ascontiguousarray(np.broadcast_to(inp["router_b"][L][:, None, :], (len(L), 128, NE)))
    wg = inp["exp_w_gu"][L]
    o["w_gu"] = np.ascontiguousarray(wg.reshape(len(L), ne, D, KC, 128, 2).transpose(0, 1, 2, 3, 5, 4)).reshape(len(L), ne, D, 2 * D)
    bg = inp["exp_b_gu"][L].reshape(len(L), ne, KC, 128, 2).transpose(0, 3, 1, 2, 4)
    o["b_gu"] = np.ascontiguousarray(bg).reshape(len(L), 128, ne, 16)
    o["w_dn"] = np.ascontiguousarray(inp["exp_w_dn"][L])
    o["b_dn"] = np.ascontiguousarray(inp["exp_b_dn"][L])
    o["final_g"] = np.ascontiguousarray(inp["final_g"].reshape(KC, 128).T)
    sel = np.zeros((NE, NE, 128), np.float32)
    for e in range(NE):
        sel[e, e, :] = 1.0
    o["sel"] = sel.reshape(NE, NE * 128)
    o["ident"] = np.eye(128, dtype=np.float32)
    return o


def prep_even(inp, js):
    js = list(js)
    o = {}
    if not js:
        js = [0]
    n = len(js)
    W = inp["ev_w_in"][js]
    pool, q, k, v = W[:, :, 0:256], W[:, :, 256:1024], W[:, :, 1024:1216], W[:, :, 1216:1408]
    perm = np.arange(64).reshape(2, 2, 16)[:, ::-1, :].reshape(64)
    qsw = q.reshape(n, D, 12, 64)[..., perm].reshape(n, D, 768)
    kh = k.reshape(n, D, 3, 64)
    ksw = kh[..., perm]
    k2 = np.concatenate([kh, kh], -1).reshape(n, D, 384)
    ksw2 = np.concatenate([ksw, ksw], -1).reshape(n, D, 384)
    o["ev_w"] = np.ascontiguousarray(np.concatenate([pool, q, qsw, k2, ksw2, v], -1))
    o["ev_wout"] = np.ascontiguousarray(inp["ev_w_out"][js])
    pw = np.zeros((n, 2, 128, 128), np.float32)
    for c in range(2):
        for g in range(2):
            pw[:, c, g * 64:(g + 1) * 64, g * 64:(g + 1) * 64] = inp["pool_w"][js][:, 2 * c + g]
    o["pw_blk"] = pw
    o["pscale"] = np.ascontiguousarray(inp["pool_scale"][js].reshape(n, 2, 128).transpose(0, 2, 1))
    o["sinkcol"] = np.ascontiguousarray(np.broadcast_to(inp["att_sink"][js][:, None, :], (n, 128, 12)))
    return o


def prep_odd(inp, js):
    js = list(js)
    if not js:
        js = [0]
    n = len(js)
    o = {}
    o["od_w"] = np.ascontiguousarray(inp["od_w_in"][js])
    o["od_wout"] = np.ascontiguousarray(inp["od_w_out"][js])
    mu = inp["rw_mu"][js]
    mup = np.zeros((n, 2, 22 * 128), np.float32)
    mup[:, :, 0:2720] = mu
    o["mu_l"] = np.ascontiguousarray(mup.reshape(n, 2, 22, 128).transpose(0, 3, 2, 1))
    o["rw_w2"] = np.ascontiguousarray(inp["rw_w2"][js].reshape(n, 128, 768))
    o["rw_a2"] = np.ascontiguousarray(inp["rw_a2"][js].reshape(n, 128, 768))
    o["rw_g2"] = np.ascontiguousarray(inp["rw_g2"][js])
    vecs = [inp["rw_w0"][js][:, 0], inp["rw_w0"][js][:, 1], inp["rw_a0"][js][:, 0], inp["rw_a0"][js][:, 1],
            inp["rw_k_k"][js], inp["rw_k_a"][js], inp["rw_r_k"][js].reshape(n, 768), inp["rw_gn_g"][js], inp["rw_gn_b"][js]]
    pv = np.stack(vecs, axis=-1)
    o["rw_pv"] = np.ascontiguousarray(pv.reshape(n, 6, 128, 9).transpose(0, 2, 1, 3))
    return o


def const_tables():
    import ml_dtypes
    o = {}
    blk = np.zeros((128, 128), np.float32)
    blk[0:64, 0:64] = 1.0
    blk[64:128, 64:128] = 1.0
    o["blk1"] = blk
    rr = np.arange(128)[:, None]
    cc = np.arange(128)[None, :]
    rm = np.stack([(cc > rr), (cc >= rr), (cc < rr), (cc <= rr)]).astype(np.float32) * blk[None]
    o["rmask"] = np.ascontiguousarray(rm)
    c64 = np.arange(64)
    ph = 2.0 * np.pi * ((c64[:, None] * c64[None, :]) % 64) / 64.0
    d64 = np.zeros((2, 128, 128), np.float64)
    for g in range(2):
        d64[0, g * 64:(g + 1) * 64, g * 64:(g + 1) * 64] = np.cos(ph)
        d64[1, g * 64:(g + 1) * 64, g * 64:(g + 1) * 64] = np.sin(ph)
    o["dft64"] = d64.astype(ml_dtypes.bfloat16)
    for nm, T in (("dftx", SEQ), ("dftc", CTX)):
        t = np.arange(T, dtype=np.int64)
        th = 2.0 * np.pi * ((t[:, None] * t[None, :]) % T) / T
        sc = 1.0 / np.sqrt(64.0 * T)
        tab = np.stack([np.cos(th) * sc, -np.sin(th) * sc])
        tab = tab.reshape(2, T // 128, 128, T).transpose(0, 2, 1, 3)
        o[nm] = np.ascontiguousarray(tab).astype(ml_dtypes.bfloat16)
    t = np.arange(SEQ)
    row = (t // 64).astype(np.float32)
    colp = (t % 64).astype(np.float32)
    inv_freq = (np.float32(10000.0) ** (-np.arange(16, dtype=np.float32) / np.float32(16))).astype(np.float32)
    C = np.zeros((128, SEQ), np.float32)
    Sg = np.zeros((128, SEQ), np.float32)
    for p in range(128):
        dd = p % 64
        a, s_, f = dd // 32, (dd % 32) // 16, dd % 16
        ang = ((row if a == 0 else colp) * inv_freq[f]).astype(np.float32)
        C[p] = np.cos(ang)
        Sg[p] = (-1.0 if s_ == 0 else 1.0) * np.sin(ang)
    o["ropeC"], o["ropeS"] = C, Sg
    qi = np.arange(128)[:, None]
    ki = np.arange(128)[None, :]
    NEG = np.float32(-30000.0)
    mlow = np.where(ki >= qi, 0.0, NEG).astype(np.float32)
    mup = np.where(ki <= qi, 0.0, NEG).astype(np.float32)
    am = np.zeros((3, 128, 640), np.float32)
    am[0, :, 384:512] = mup
    am[1, :, 256:384] = mlow
    am[1, :, 512:640] = mup
    am[2, :, 256:384] = mlow
    o["amask"] = am
    for nm, T in (("inv_x", SEQ), ("inv_c", CTX)):
        inv = np.zeros((128, 2, T), np.float32)
        tt = np.arange(T)
        for c in range(2):
            for g in range(2):
                w = (2, 4, 8, 16)[2 * c + g]
                lo = np.clip(tt - w // 2, 0, T)
                hi = np.clip(tt - w // 2 + w, 0, T)
                inv[g * 64:(g + 1) * 64, c, :] = (1.0 / (hi - lo).astype(np.float32))[None, :]
        o[nm] = inv
    return o


def prep_core(inp, core, h_x=None, h_c=None):
    o = {}
    bs = slice(core * NB, (core + 1) * NB)
    x = inp["x"][bs] if h_x is None else h_x
    c = inp["ctx"][bs] if h_c is None else h_c
    tok = np.concatenate([c, x], axis=1).reshape(NT, D)
    o["h0"] = fm(tok)
    cc = np.zeros((4, D), np.float32)
    cc[0:NB] = inp["c"][bs]
    cc[2] = inp["c_ctx"]
    o["cT"] = np.ascontiguousarray(cc.reshape(4, KC, 128).transpose(2, 1, 0))
    return o


def unfm(a):
    t = a.transpose(2, 1, 0).reshape(NB, NTB, D)
    return t[:, CTX:], t[:, :CTX]


_PROG = {}


def kernel(**inputs):
    inputs = {k: np.asarray(v) for k, v in inputs.items()}
    if "full" not in _PROG:
        _PROG["full"] = Prog()
    prog = _PROG["full"]
    shared = prep_shared(inputs, range(DEPTH))
    shared.update(prep_even(inputs, [0, 1]))
    shared.update(prep_odd(inputs, [0, 1]))
    shared.update(const_tables())
    in_maps = []
    for c in range(NCORES):
        m = dict(shared)
        m.update(prep_core(inputs, c))
        in_maps.append(m)
    res = run_bass_kernel_spmd(prog.nc, in_maps, core_ids=list(range(NCORES)))
    outs = [unfm(np.asarray(r["out"]))[0] for r in res.results]
    return np.ascontiguousarray(np.concatenate(outs, axis=0).astype(np.float32))
```
